# Optimizing a Trainium2 kernel written in Bass

```python
import jax, jax.numpy as jnp
from jax import lax
import numpy as np

D_MODEL = 1024
BATCH = 8
SEQ = 2048
DEPTH = 4

HEAD_DIM = 64
SB_HEADS = 8
SB_WIDTH = SB_HEADS * HEAD_DIM
POOL_WINDOWS = (2, 4, 8, 16)
POOL_WIDTH = D_MODEL // 2
POOL_GROUP = POOL_WIDTH // len(POOL_WINDOWS)
EVEN_IN = 3 * SB_WIDTH + POOL_WIDTH
EVEN_MIX = SB_WIDTH + POOL_WIDTH
MOBA_HEADS = D_MODEL // HEAD_DIM
MOBA_WIDTH = MOBA_HEADS * HEAD_DIM
MOBA_BLOCK = 256
MOBA_TOPK = 3
MOBA_QCHUNK = 16
Q_BLOCK = 128
D_FF = 2816
N_EVEN = (DEPTH + 1) // 2
N_ODD = DEPTH // 2
RMS_EPS = 1e-6

kernel_name = "hybrid_stickbreak_pool_moba_macaron"


def rmsnorm(x, g):
    xf = x.astype(jnp.float32)
    y = xf * lax.rsqrt(jnp.mean(xf * xf, axis=-1, keepdims=True) + RMS_EPS)
    return (y * g.astype(jnp.float32)).astype(x.dtype)


def swiglu(h, w_gate, w_up, w_down):
    return (jax.nn.silu(h @ w_gate) * (h @ w_up)) @ w_down


def split_heads(t, n):
    b, s, _ = t.shape
    return t.reshape(b, s, n, HEAD_DIM).transpose(0, 2, 1, 3)


def merge_heads(t):
    b, h, s, dh = t.shape
    return t.transpose(0, 2, 1, 3).reshape(b, s, h * dh)


def alibi_slopes(n):
    return jnp.asarray(2.0 ** (-8.0 * np.arange(1, n + 1) / n), dtype=jnp.float32)


def stick_breaking_attention(q, k, v):
    s_len = q.shape[2]
    scale = HEAD_DIM ** -0.5
    outs = []
    for i in range(s_len // Q_BLOCK):
        q0 = i * Q_BLOCK
        kv_len = q0 + Q_BLOCK
        qb = q[:, :, q0:kv_len]
        kb = k[:, :, :kv_len]
        vb = v[:, :, :kv_len]
        z = jnp.einsum('bhtd,bhsd->bhts', qb, kb).astype(jnp.float32) * scale
        tpos = q0 + jnp.arange(Q_BLOCK)[:, None]
        spos = jnp.arange(kv_len)[None, :]
        past = spos < tpos
        log_1m = jnp.where(past, jax.nn.log_sigmoid(-z), 0.0)
        later = lax.cumsum(log_1m, axis=3, reverse=True) - log_1m
        a = jnp.where(past, jnp.exp(jax.nn.log_sigmoid(z) + later), 0.0)
        outs.append(jnp.einsum('bhts,bhsd->bhtd', a.astype(v.dtype), vb))
    return jnp.concatenate(outs, axis=2)


def multiscale_pool(u, w_pool, pool_scale):
    b, s, _ = u.shape
    uf = u.astype(jnp.float32)
    cs = jnp.concatenate([jnp.zeros((b, 1, POOL_WIDTH), jnp.float32), jnp.cumsum(uf, axis=1)], axis=1)
    t = jnp.arange(s)
    groups = []
    for g, w in enumerate(POOL_WINDOWS):
        sl = slice(g * POOL_GROUP, (g + 1) * POOL_GROUP)
        start = jnp.maximum(t + 1 - w, 0)
        win_sum = cs[:, 1:, sl] - cs[:, start, sl]
        count = jnp.minimum(t + 1, w).astype(jnp.float32)[None, :, None]
        groups.append(win_sum / count - uf[:, :, sl])
    pooled = jnp.stack(groups, axis=2).astype(u.dtype)
    mixed = jnp.einsum('bsgc,gcd->bsgd', pooled, w_pool).reshape(b, s, POOL_WIDTH)
    return mixed * pool_scale


def moba_attention(q, k, v):
    b, h, s, dh = q.shape
    nb = -(-s // MOBA_BLOCK)
    pad = nb * MOBA_BLOCK - s
    kp = jnp.pad(k, ((0, 0), (0, 0), (0, pad), (0, 0)))
    vp = jnp.pad(v, ((0, 0), (0, 0), (0, pad), (0, 0)))
    k_blocks = kp.reshape(b, h, nb, MOBA_BLOCK, dh)
    v_blocks = vp.reshape(b, h, nb, MOBA_BLOCK, dh)
    k_mean = jnp.mean(k_blocks.astype(jnp.float32), axis=3).astype(k.dtype)
    gate = jnp.einsum('bhtd,bhnd->bhtn', q, k_mean).astype(jnp.float32)
    qblk_all = jnp.arange(s) // MOBA_BLOCK
    past_blk = jnp.arange(nb)[None, :] < qblk_all[:, None]
    gate = jnp.where(past_blk, gate, -jnp.inf)
    topk = min(MOBA_TOPK, nb)
    _, sel = lax.top_k(gate, topk)
    scale = dh ** -0.5
    slopes = alibi_slopes(h)
    bi = jnp.arange(b)[:, None, None, None]
    hi = jnp.arange(h)[None, :, None, None]

    def chunk(c):
        t0 = c * MOBA_QCHUNK
        qc = lax.dynamic_slice_in_dim(q, t0, MOBA_QCHUNK, axis=2)
        sel_c = lax.dynamic_slice_in_dim(sel, t0, MOBA_QCHUNK, axis=2)
        tq = t0 + jnp.arange(MOBA_QCHUNK)
        blk0 = (t0 // MOBA_BLOCK) * MOBA_BLOCK
        k_own = lax.dynamic_slice_in_dim(kp, blk0, MOBA_BLOCK, axis=2)
        v_own = lax.dynamic_slice_in_dim(vp, blk0, MOBA_BLOCK, axis=2)
        own_pos = blk0 + jnp.arange(MOBA_BLOCK)
        k_sel = k_blocks[bi, hi, sel_c]
        v_sel = v_blocks[bi, hi, sel_c]
        sel_pos = sel_c[..., None] * MOBA_BLOCK + jnp.arange(MOBA_BLOCK)
        sel_ok = (sel_c < (tq // MOBA_BLOCK)[:, None])[..., None]
        d_own = (tq[:, None] - own_pos[None, :]).astype(jnp.float32)
        s_own = (jnp.einsum('bhtd,bhsd->bhts', qc, k_own).astype(jnp.float32) * scale
                 - slopes[None, :, None, None] * d_own)
        s_own = jnp.where(own_pos[None, :] <= tq[:, None], s_own, -jnp.inf)
        d_sel = (tq[:, None, None] - sel_pos).astype(jnp.float32)
        s_sel = (jnp.einsum('bhtd,bhtksd->bhtks', qc, k_sel).astype(jnp.float32) * scale
                 - slopes[None, :, None, None, None] * d_sel)
        s_sel = jnp.where(sel_ok, s_sel, -jnp.inf).reshape(b, h, MOBA_QCHUNK, topk * MOBA_BLOCK)
        p = jax.nn.softmax(jnp.concatenate([s_own, s_sel], axis=-1), axis=-1)
        p_own = p[..., :MOBA_BLOCK].astype(v.dtype)
        p_sel = p[..., MOBA_BLOCK:].reshape(b, h, MOBA_QCHUNK, topk, MOBA_BLOCK).astype(v.dtype)
        return (jnp.einsum('bhts,bhsd->bhtd', p_own, v_own)
                + jnp.einsum('bhtks,bhtksd->bhtd', p_sel, v_sel))

    outs = lax.map(chunk, jnp.arange(s // MOBA_QCHUNK))
    return outs.transpose(1, 2, 0, 3, 4).reshape(b, h, s, dh)


def even_mixer(h, w_in, w_pool, pool_scale, w_out):
    proj = h @ w_in
    q, k, v, u = jnp.split(proj, [SB_WIDTH, 2 * SB_WIDTH, 3 * SB_WIDTH], axis=-1)
    a = stick_breaking_attention(split_heads(q, SB_HEADS), split_heads(k, SB_HEADS), split_heads(v, SB_HEADS))
    p = multiscale_pool(u, w_pool, pool_scale)
    return jnp.concatenate([merge_heads(a), p], axis=-1) @ w_out


def odd_mixer(h, w_qkv, w_o):
    q, k, v = jnp.split(h @ w_qkv, 3, axis=-1)
    o = moba_attention(split_heads(q, MOBA_HEADS), split_heads(k, MOBA_HEADS), split_heads(v, MOBA_HEADS))
    return merge_heads(o) @ w_o


def setup_inputs(seed: int = 0) -> dict:
    key = jax.random.key(seed)
    ks = jax.random.split(key, 20)
    f32 = jnp.float32

    def w(k, shape, fan_in):
        return jax.random.normal(k, shape, f32) * (fan_in ** -0.5)

    def gain(k, shape):
        return 1.0 + 0.05 * jax.random.normal(k, shape, f32)

    return {
        "x": jax.random.normal(ks[0], (BATCH, SEQ, D_MODEL), f32),
        "norm_ffn1": gain(ks[1], (DEPTH, D_MODEL)),
        "ffn1_gate": w(ks[2], (DEPTH, D_MODEL, D_FF), D_MODEL),
        "ffn1_up": w(ks[3], (DEPTH, D_MODEL, D_FF), D_MODEL),
        "ffn1_down": w(ks[4], (DEPTH, D_FF, D_MODEL), D_FF),
        "norm_mix": gain(ks[5], (DEPTH, D_MODEL)),
        "norm_ffn2": gain(ks[6], (DEPTH, D_MODEL)),
        "ffn2_gate": w(ks[7], (DEPTH, D_MODEL, D_FF), D_MODEL),
        "ffn2_up": w(ks[8], (DEPTH, D_MODEL, D_FF), D_MODEL),
        "ffn2_down": w(ks[9], (DEPTH, D_FF, D_MODEL), D_FF),
        "even_w_in": w(ks[10], (N_EVEN, D_MODEL, EVEN_IN), D_MODEL),
        "even_w_pool": w(ks[11], (N_EVEN, len(POOL_WINDOWS), POOL_GROUP, POOL_GROUP), POOL_GROUP),
        "even_pool_scale": gain(ks[12], (N_EVEN, POOL_WIDTH)),
        "even_w_out": w(ks[13], (N_EVEN, EVEN_MIX, D_MODEL), EVEN_MIX),
        "odd_w_qkv": w(ks[14], (N_ODD, D_MODEL, 3 * MOBA_WIDTH), D_MODEL),
        "odd_w_o": w(ks[15], (N_ODD, MOBA_WIDTH, D_MODEL), MOBA_WIDTH),
        "norm_final": gain(ks[16], (D_MODEL,)),
    }


def reference(x, norm_ffn1, ffn1_gate, ffn1_up, ffn1_down, norm_mix, norm_ffn2,
              ffn2_gate, ffn2_up, ffn2_down, even_w_in, even_w_pool, even_pool_scale,
              even_w_out, odd_w_qkv, odd_w_o, norm_final):
    for layer in range(DEPTH):
        x = x + 0.5 * swiglu(rmsnorm(x, norm_ffn1[layer]), ffn1_gate[layer], ffn1_up[layer], ffn1_down[layer])
        h = rmsnorm(x, norm_mix[layer])
        i = layer // 2
        if layer % 2 == 0:
            x = x + even_mixer(h, even_w_in[i], even_w_pool[i], even_pool_scale[i], even_w_out[i])
        else:
            x = x + odd_mixer(h, odd_w_qkv[i], odd_w_o[i])
        x = x + 0.5 * swiglu(rmsnorm(x, norm_ffn2[layer]), ffn2_gate[layer], ffn2_up[layer], ffn2_down[layer])
    return rmsnorm(x, norm_final)
```

```python
import numpy as np
import ml_dtypes
import concourse.bass as bass
import concourse.mybir as mybir
from concourse.bass_utils import run_bass_kernel_spmd

F32 = mybir.dt.float32
BF16 = mybir.dt.bfloat16
AF = mybir.ActivationFunctionType
ALU = mybir.AluOpType
AX = mybir.AxisListType

S = 2048
D = 1024
DFF = 2816
NT = S // 128
NC_ = D // 128
NFF = DFF // 128
DEPTH = 4
EPS = 1e-6
NEG = -30000.0

COMPUTE = ("pe", "act", "dve")
SEM_ROLL = 2000


class Prog:
    def __init__(self, nc):
        self.nc = nc
        self.ops = []
        self.last_writer = {}
        self.readers = {}
        self.dma_cnt = {}
        self.fence_at = []

    def _deps(self, reads, writes):
        deps = set()
        for r in reads:
            w = self.last_writer.get(r)
            if w is not None:
                deps.add(w)
        for r in writes:
            w = self.last_writer.get(r)
            if w is not None:
                deps.add(w)
            last_by_eng = {}
            for rd in self.readers.get(r, ()):
                o = self.ops[rd]
                if o["dma_key"] is not None:
                    deps.add(rd)
                else:
                    last_by_eng[o["eng"]] = max(last_by_eng.get(o["eng"], -1), rd)
            deps.update(last_by_eng.values())
        return deps

    def op(self, eng, fn, reads=(), writes=(), dma_key=None):
        idx = len(self.ops)
        deps = self._deps(reads, writes)
        dep_vals = {}
        for d in deps:
            dk = self.ops[d]["dma_key"]
            if dk is not None:
                dep_vals[d] = 16 * self.dma_cnt[dk]
        if dma_key is not None:
            self.dma_cnt[dma_key] = self.dma_cnt.get(dma_key, 0) + 1
        self.ops.append(dict(eng=eng, fn=fn, deps=deps, dep_vals=dep_vals, dma_key=dma_key,
                             signal=False, sig=None, nfence=len(self.fence_at)))
        for r in reads:
            self.readers.setdefault(r, []).append(idx)
        for r in writes:
            self.last_writer[r] = idx
            self.readers[r] = []
        return idx

    def fence(self):
        self.fence_at.append(len(self.ops))

    def finalize(self, sem_alloc):
        ops = self.ops
        fence_last = []
        for fpos in self.fence_at:
            last = {}
            for e in COMPUTE:
                for i in range(fpos - 1, -1, -1):
                    if ops[i]["eng"] == e and ops[i]["dma_key"] is None:
                        last[e] = i
                        break
            fence_last.append(last)
        self.fence_last = fence_last
        for last in fence_last:
            for i in last.values():
                ops[i]["signal"] = True
        for i, o in enumerate(ops):
            keep = set()
            for d in o["deps"]:
                p = ops[d]
                if p["dma_key"] is None:
                    if p["eng"] == o["eng"] and o["dma_key"] is None:
                        if o["eng"] == "pe":
                            continue
                    p["signal"] = True
                keep.add(d)
            o["deps"] = keep
        cnt = {}
        semidx = {}
        self.eng_sems = {}
        for o in ops:
            if o["dma_key"] is not None:
                continue
            if not o["signal"]:
                continue
            e = o["eng"]
            k = semidx.get(e, 0)
            c = cnt.get((e, k), 0)
            if c >= SEM_ROLL:
                k += 1
                semidx[e] = k
                c = 0
            c += 1
            cnt[(e, k)] = c
            if (e, k) not in self.eng_sems:
                self.eng_sems[(e, k)] = sem_alloc("p_%s_%d" % (e, k))
            o["sig"] = (self.eng_sems[(e, k)], c)
        self.dma_sems = {}
        run = {}
        for o in ops:
            dk = o["dma_key"]
            if dk is None:
                continue
            if dk not in self.dma_sems:
                self.dma_sems[dk] = sem_alloc("d_%s" % (str(dk),))
            run[dk] = run.get(dk, 0) + 1
            o["sig"] = (self.dma_sems[dk], 16 * run[dk])

    def emit(self, eng_name, e):
        ops = self.ops
        waited = {}
        nf_done = 0

        def wait(sem, val):
            key = id(sem)
            if waited.get(key, 0) >= val:
                return
            waited[key] = val
            e.wait_ge(sem, val)

        n_wait0 = 0
        for i, o in enumerate(ops):
            if o["eng"] != eng_name:
                continue
            if eng_name in COMPUTE:
                while nf_done < o["nfence"]:
                    for oe, li in self.fence_last[nf_done].items():
                        sem, val = ops[li]["sig"]
                        wait(sem, val)
                    nf_done += 1
            for d in sorted(o["deps"]):
                p = ops[d]
                sem, val = p["sig"]
                if p["dma_key"] is not None:
                    val = o["dep_vals"][d]
                wait(sem, val)
            ins = o["fn"](e)
            if o["dma_key"] is not None:
                ins.then_inc(o["sig"][0], 16)
            elif o["signal"]:
                ins.then_inc(o["sig"][0], 1)

    def final_waits(self, e):
        run = {}
        for o in self.ops:
            if o["dma_key"] is not None:
                run[o["dma_key"]] = o["sig"]
        for dk, (sem, val) in run.items():
            e.wait_ge(sem, val)


def make_consts():
    bf = ml_dtypes.bfloat16
    c = {}
    c["c_ident"] = np.eye(128, dtype=np.float32).astype(bf)
    c["c_ones"] = np.ones((128, 128), np.float32).astype(bf)
    j = np.arange(128)[:, None]
    col = np.arange(897)[None, :]
    c["c_mask_strict"] = (j + 384 < col).astype(np.float32).astype(bf)
    c["c_bias_incl"] = np.where(j + 384 <= col, 0.0, NEG).astype(np.float32).astype(bf)
    s_ = np.arange(128)[None, :]
    c["c_negtri"] = (-(j >= s_).astype(np.float32)).astype(bf)
    c["c_negones"] = (-np.ones((128, 128), np.float32)).astype(bf)
    band = np.zeros((4, 3, 128, 128), np.float32)
    sidx = np.arange(128)[:, None]
    tidx = np.arange(128)[None, :]
    for g, w in enumerate((2, 4, 8, 16)):
        d0 = tidx - sidx
        band[g, 0] = np.where((d0 >= 0) & (d0 < w), 1.0 / w, 0.0) - (d0 == 0)
        d1 = tidx + 128 - sidx
        band[g, 1] = np.where((d1 >= 0) & (d1 < w), 1.0 / w, 0.0)
        cnt = np.minimum(tidx + 1, w).astype(np.float32)
        band[g, 2] = np.where((d0 >= 0) & (d0 < w), 1.0 / cnt, 0.0) - (d0 == 0)
    c["c_band"] = np.ascontiguousarray(band.transpose(2, 0, 1, 3)).astype(bf)
    slopes = (2.0 ** (-8.0 * np.arange(1, 17) / 16)).astype(np.float32)
    en = np.zeros((8, 8, 128), np.float32)
    for n in range(8):
        en[n, n, :] = 1.0
    c["c_en"] = en.astype(bf)
    p = np.arange(128, dtype=np.float32)[:, None, None]
    off = (np.arange(19, dtype=np.float32) - 3.0)[None, None, :] * 128.0
    c["c_albias"] = (slopes[None, :, None] * (p - off)).astype(np.float32)
    tl = ((np.arange(16) % 4) * 128.0)[None, :, None] + p
    c["c_altab"] = (-(slopes[None, None, :]) * tl).astype(np.float32)
    blk = (np.arange(16) // 2)[:, None]
    n = np.arange(8)[None, :]
    past = np.where(n < blk, 0.0, NEG).astype(np.float32)
    c["c_past"] = np.ascontiguousarray(np.broadcast_to(past[None], (128, 16, 8))).astype(np.float32)
    notown = (n != blk).astype(np.float32)
    c["c_notown"] = np.ascontiguousarray(np.broadcast_to(notown[None], (128, 16, 8))).astype(np.float32)
    return c


CONST_DT = {"c_ident": BF16, "c_ones": BF16, "c_mask_strict": BF16, "c_bias_incl": BF16,
            "c_negtri": BF16, "c_negones": BF16, "c_band": BF16, "c_en": BF16, "c_albias": F32,
            "c_altab": F32, "c_past": F32, "c_notown": F32}

WEIGHT_SHAPES = {
    "norm_ffn1": (4, D), "ffn1_gate": (4, D, DFF), "ffn1_up": (4, D, DFF), "ffn1_down": (4, DFF, D),
    "norm_mix": (4, D), "norm_ffn2": (4, D), "ffn2_gate": (4, D, DFF), "ffn2_up": (4, D, DFF),
    "ffn2_down": (4, DFF, D), "even_w_in": (2, D, 2048), "even_w_pool": (2, 4, 128, 128),
    "even_pool_scale": (2, 512), "even_w_out": (2, D, D), "odd_w_qkv": (2, D, 3072),
    "odd_w_o": (2, D, D), "norm_final": (D,),
}

RING_K = 4
DEBUG = {}
WORK_BYTES = 28 * 1024


def build(n_layers=DEPTH, stages=None, dbg_stage=None):
    from contextlib import ExitStack
    nc = bass.Bass("TRN2", target_bir_lowering=False)
    P = Prog(nc)
    dr = {}
    dr["x"] = nc.dram_tensor("x", [S, D], F32, kind="ExternalInput").ap()
    for name, shp in WEIGHT_SHAPES.items():
        dr[name] = nc.dram_tensor(name, list(shp), F32, kind="ExternalInput").ap()
    consts = make_consts()
    for name, arr in consts.items():
        dr[name] = nc.dram_tensor(name, list(arr.shape), CONST_DT[name], kind="ExternalInput").ap()
    out_d = nc.dram_tensor("out", [S, D], F32, kind="ExternalOutput").ap()

    st = ExitStack()
    with st:
        def T(name, shape, dt):
            return st.enter_context(nc.sbuf_tensor(name, list(shape), dt))

        x_sb = T("x_sb", [128, NT, D], F32)
        hT = T("hT", [128, NC_, S], BF16)
        catT = T("catT", [128, NC_, S], BF16)
        ring = [T("ring%d" % k, [128, 8, 512], BF16) for k in range(RING_K)]
        work = T("work", [128, WORK_BYTES // 2], BF16)
        ident = T("ident", [128, 128], BF16)
        ones_bf = T("ones_bf", [128, 128], BF16)
        mask_strict = T("mask_strict", [128, 897], BF16)
        bias_incl = T("bias_incl", [128, 897], BF16)
        negtri = T("negtri", [128, 128], BF16)
        negones = T("negones", [128, 128], BF16)
        band = T("band", [128, 4, 3, 128], BF16)
        en_sb = T("en_sb", [40, 8, 128], BF16)
        albias = T("albias", [128, 16, 19], F32)
        altab = T("altab", [128, 16, 16], F32)
        past_sb = T("past_sb", [128, 16, 8], F32)
        notown_sb = T("notown_sb", [128, 16, 8], F32)
        gT = T("gT", [128, 12, 8], F32)
        wpool = T("wpool", [128, 4, 128], BF16)
        pscale = T("pscale", [128, 4], F32)
        ss = T("ss", [128, NT], F32)
        gfin_t = T("gfin", [128, D], F32)
        gfin = gfin_t[:]
        rstd = T("rstd", [128, NT], F32)
        ps = [st.enter_context(nc.psum_tensor("ps%d" % k, [128, 512], F32)) for k in range(8)]
        print("sbuf bytes remaining:", nc.sbuf_bytes_remaining)

        woff = [0]

        def carve_reset():
            woff[0] = 0

        def carve(shape, dt):
            esz = 4 if dt == F32 else 2
            n = int(np.prod(shape[1:]))
            nb = n * esz
            o = woff[0]
            assert o % 4 == 0
            woff[0] = o + ((nb + 31) // 32) * 32
            assert woff[0] <= WORK_BYTES, (woff[0], WORK_BYTES)
            ap = work[0:shape[0], o // 2:(o + nb) // 2]
            if dt == F32:
                ap = ap.bitcast(F32)
            if len(shape) == 3:
                ap = ap.rearrange("p (a b) -> p a b", a=shape[1])
            elif len(shape) == 4:
                ap = ap.rearrange("p (a b c) -> p a b c", a=shape[1], b=shape[2])
            return ap

        def ld(dst, src, key, eng="sp", **kw):
            P.op(eng, lambda e: e.dma_start(out=dst, in_=src, **kw), writes=[key], dma_key=key)

        for t4 in range(4):
            P.op("sp", lambda e, t4=t4: e.dma_start(
                out=x_sb[:, 4 * t4:4 * t4 + 4, :],
                in_=dr["x"][t4 * 512:(t4 + 1) * 512, :].rearrange("(i p) d -> p i d", p=128)),
                writes=[("x", i, h) for i in range(4 * t4, 4 * t4 + 4) for h in range(2)],
                dma_key=("xin", t4))
        ld(ident[:], dr["c_ident"], "ident")
        ld(ones_bf[:], dr["c_ones"], "ones_bf")
        ld(mask_strict[:], dr["c_mask_strict"], "mask_strict")
        ld(bias_incl[:], dr["c_bias_incl"], "bias_incl")
        ld(negtri[:], dr["c_negtri"], "negtri")
        ld(negones[:], dr["c_negones"], "negones")
        ld(band[:], dr["c_band"], "band")
        P.op("sp", lambda e: e.dma_start(out=en_sb[0:8], in_=dr["c_en"]), writes=["en_sb"], dma_key="en_sb")
        P.op("sp", lambda e: e.dma_start(out=en_sb[32:40], in_=dr["c_en"]), writes=["en_sb"], dma_key="en_sb")
        ld(albias[:], dr["c_albias"], "albias")
        ld(altab[:], dr["c_altab"], "altab")
        ld(past_sb[:], dr["c_past"], "past_sb")
        ld(notown_sb[:], dr["c_notown"], "notown_sb")
        for k, nm in enumerate(("norm_ffn1", "norm_mix", "norm_ffn2")):
            for l in range(4):
                P.op("sp", lambda e, k=k, l=l, nm=nm: e.dma_start(
                    out=gT[:, k * 4 + l, :], in_=dr[nm][l].rearrange("(c p) -> p c", p=128),
                    allow_slow_non_contiguous=True), writes=["gT"], dma_key="gT")

        ring_ctr = [0]

        def ring_next():
            k = ring_ctr[0] % RING_K
            ring_ctr[0] += 1
            return k

        def wload(k, dst, src):
            P.op("pool", lambda e: e.dma_start(out=dst, in_=src), writes=[("ring", k)],
                 dma_key=("ring", k))

        def norm_phase(gidx):
            carve_reset()
            junk = carve([128, D], BF16)
            xn = [carve([128, D], BF16) for _ in range(4)]
            for i in range(NT):
                P.op("act", lambda e, i=i: e.activation(out=junk, in_=x_sb[:, i, :], func=AF.Square,
                                                        accum_out=ss[:, i:i + 1]),
                     reads=[("x", i, 0), ("x", i, 1)], writes=[("ss", i)])
            P.op("act", lambda e: e.activation(out=rstd[:], in_=ss[:], func=AF.Sqrt, scale=1.0 / D, bias=EPS),
                 reads=[("ss", i) for i in range(NT)], writes=["rstd"])
            P.op("dve", lambda e: e.reciprocal(out=rstd[:], in_=rstd[:]), reads=["rstd"], writes=["rstd"])
            for i in range(NT):
                xb = xn[i % 4]
                if i % 2 == 0:
                    P.op("act", lambda e, i=i, xb=xb: e.activation(out=xb, in_=x_sb[:, i, :], func=AF.Copy,
                                                                   scale=rstd[:, i:i + 1]),
                         reads=[("x", i, 0), ("x", i, 1), "rstd"], writes=[("xn", i % 4)])
                else:
                    P.op("dve", lambda e, i=i, xb=xb: e.tensor_scalar(out=xb, in0=x_sb[:, i, :], scalar1=rstd[:, i:i + 1],
                                                                      scalar2=None, op0=ALU.mult),
                         reads=[("x", i, 0), ("x", i, 1), "rstd"], writes=[("xn", i % 4)])
                pb = 4 + (i % 4)
                psT = ps[pb][:].bitcast(BF16)
                for c in range(NC_):
                    P.op("pe", lambda e, c=c, xb=xb, psT=psT: e.transpose(
                        out=psT[:, c * 128:(c + 1) * 128], in_=xb[:, c * 128:(c + 1) * 128],
                        identity=ident[:]),
                        reads=[("xn", i % 4), "ident"], writes=[("ps", pb)])
                P.op("dve", lambda e, i=i, psT=psT: e.tensor_tensor(
                    out=hT[:, :, i * 128:(i + 1) * 128],
                    in0=psT.rearrange("p (c t) -> p c t", c=NC_),
                    in1=gT[:, gidx, :].unsqueeze(2).to_broadcast([128, NC_, 128]), op=ALU.mult),
                    reads=[("ps", pb), "gT"], writes=[("hT", i)])

        def ffn_phase(wg, wu, wd):
            carve_reset()
            sil = [carve([128, 512], F32) for _ in range(2)]
            groups = [(0, 8), (8, 8), (16, 6)]
            nsil = 0
            for (f0, n) in groups:
                for sub0 in range(0, n, 4):
                    ns = min(4, n - sub0)
                    kg = ring_next()
                    col0 = (f0 + sub0) * 128
                    wload(kg, ring[kg][:, :, 0:ns * 128],
                          wg.rearrange("(c p) n -> p c n", p=128)[:, :, col0:col0 + ns * 128])
                    ku = ring_next()
                    wload(ku, ring[ku][:, :, 0:ns * 128],
                          wu.rearrange("(c p) n -> p c n", p=128)[:, :, col0:col0 + ns * 128])
                    for jj in range(ns):
                        j = sub0 + jj
                        for tb in range(4):
                            bg = tb % 2
                            bu = 2 + tb % 2
                            for c in range(NC_):
                                P.op("pe", lambda e, c=c, jj=jj, tb=tb, kg=kg, bg=bg: e.matmul(
                                    ps[bg][:], lhsT=ring[kg][:, c, jj * 128:(jj + 1) * 128],
                                    rhs=hT[:, c, tb * 512:(tb + 1) * 512], start=(c == 0), stop=(c == NC_ - 1)),
                                    reads=[("ring", kg)] + [("hT", 4 * tb + q) for q in range(4)],
                                    writes=[("ps", bg)])
                            for c in range(NC_):
                                P.op("pe", lambda e, c=c, jj=jj, tb=tb, ku=ku, bu=bu: e.matmul(
                                    ps[bu][:], lhsT=ring[ku][:, c, jj * 128:(jj + 1) * 128],
                                    rhs=hT[:, c, tb * 512:(tb + 1) * 512], start=(c == 0), stop=(c == NC_ - 1)),
                                    reads=[("ring", ku)] + [("hT", 4 * tb + q) for q in range(4)],
                                    writes=[("ps", bu)])
                            sb = sil[nsil % 2]
                            sk = nsil % 2
                            nsil += 1
                            P.op("act", lambda e, sb=sb, bg=bg: e.activation(out=sb, in_=ps[bg][:], func=AF.Silu),
                                 reads=[("ps", bg)], writes=[("sil", sk)])
                            P.op("dve", lambda e, sb=sb, bu=bu, j=j, tb=tb: e.tensor_tensor(
                                out=catT[:, j, tb * 512:(tb + 1) * 512], in0=sb, in1=ps[bu][:], op=ALU.mult),
                                reads=[("sil", sk), ("ps", bu)], writes=[("catT", j, tb)])
                kd = [ring_next(), ring_next()]
                for dh in range(2):
                    wload(kd[dh], ring[kd[dh]][:, 0:n, :],
                          wd[f0 * 128:(f0 + n) * 128, dh * 512:(dh + 1) * 512].rearrange("(j p) n -> p j n", p=128))
                for i in range(NT):
                    for dh in range(2):
                        bo = 4 + (2 * i + dh) % 2
                        for j in range(n):
                            P.op("pe", lambda e, i=i, j=j, bo=bo, kk=kd[dh], n=n: e.matmul(
                                ps[bo][:], lhsT=catT[:, j, i * 128:(i + 1) * 128], rhs=ring[kk][:, j, :],
                                start=(j == 0), stop=(j == n - 1)),
                                reads=[("ring", kd[dh]), ("catT", j, i // 4)], writes=[("ps", bo)])
                        P.op("dve", lambda e, i=i, dh=dh, bo=bo: e.scalar_tensor_tensor(
                            out=x_sb[:, i, dh * 512:(dh + 1) * 512], in0=ps[bo][:], scalar=0.5,
                            in1=x_sb[:, i, dh * 512:(dh + 1) * 512], op0=ALU.mult, op1=ALU.add),
                            reads=[("ps", bo), ("x", i, dh)], writes=[("x", i, dh)])

        def outproj_phase(w):
            kd = [ring_next(), ring_next()]
            for dh in range(2):
                wload(kd[dh], ring[kd[dh]][:, :, :],
                      w[:, dh * 512:(dh + 1) * 512].rearrange("(c p) n -> p c n", p=128))
            for i in range(NT):
                for dh in range(2):
                    bo = 4 + (2 * i + dh) % 2
                    for c in range(NC_):
                        P.op("pe", lambda e, i=i, dh=dh, c=c, bo=bo: e.matmul(
                            ps[bo][:], lhsT=catT[:, c, i * 128:(i + 1) * 128], rhs=ring[kd[dh]][:, c, :],
                            start=(c == 0), stop=(c == NC_ - 1)),
                            reads=[("ring", kd[dh]), ("catT", c, i // 4)], writes=[("ps", bo)])
                    P.op("dve", lambda e, i=i, dh=dh, bo=bo: e.tensor_tensor(
                        out=x_sb[:, i, dh * 512:(dh + 1) * 512], in0=ps[bo][:],
                        in1=x_sb[:, i, dh * 512:(dh + 1) * 512], op=ALU.add),
                        reads=[("ps", bo), ("x", i, dh)], writes=[("x", i, dh)])

        def skew_emit(items, stage_fns):
            n = len(items)
            ns = len(stage_fns)
            for i in range(n + ns - 1):
                for st_i in range(ns):
                    j = i - st_i
                    if 0 <= j < n:
                        stage_fns[st_i](items[j])

        def qkv_pair(w, qcol, kcol, vcol, qT, kT, vp):
            k = ring_next()
            wv = w.rearrange("(c p) n -> p c n", p=128)
            for jx, c0 in enumerate((qcol, kcol, vcol)):
                wload(k, ring[k][:, :, jx * 128:(jx + 1) * 128], wv[:, :, c0:c0 + 128])
            for which, dst in ((0, qT), (1, kT)):
                for tb in range(4):
                    b = tb % 2
                    for c in range(NC_):
                        P.op("pe", lambda e, c=c, tb=tb, b=b, which=which: e.matmul(
                            ps[b][:], lhsT=ring[k][:, c, which * 128:(which + 1) * 128],
                            rhs=hT[:, c, tb * 512:(tb + 1) * 512], start=(c == 0), stop=(c == NC_ - 1)),
                            reads=[("ring", k)] + [("hT", 4 * tb + q) for q in range(4)], writes=[("ps", b)])
                    if which == 0:
                        P.op("act", lambda e, tb=tb, b=b, dst=dst: e.activation(
                            out=dst[:, tb * 512:(tb + 1) * 512], in_=ps[b][:], func=AF.Copy, scale=0.125),
                            reads=[("ps", b)], writes=[("qT", tb)])
                    else:
                        P.op("dve", lambda e, tb=tb, b=b, dst=dst: e.tensor_copy(
                            out=dst[:, tb * 512:(tb + 1) * 512], in_=ps[b][:]),
                            reads=[("ps", b)], writes=[("kT", tb)])
            for i4 in range(4):
                b = 2 + i4 % 2
                for q in range(4):
                    i = 4 * i4 + q
                    for c in range(NC_):
                        P.op("pe", lambda e, c=c, i=i, q=q, b=b: e.matmul(
                            ps[b][:, q * 128:(q + 1) * 128], lhsT=hT[:, c, i * 128:(i + 1) * 128],
                            rhs=ring[k][:, c, 256:384], start=(c == 0), stop=(c == NC_ - 1)),
                            reads=[("ring", k), ("hT", i)], writes=[("ps", b)])
                P.op("dve", lambda e, i4=i4, b=b: e.tensor_copy(
                    out=vp[:, 4 * i4:4 * i4 + 4, :], in_=ps[b][:].rearrange("p (q n) -> p q n", q=4)),
                    reads=[("ps", b)], writes=[("vp", i4)])

        def even_phase(li):
            w_in = dr["even_w_in"][li]
            P.op("pool", lambda e: e.dma_start(out=wpool[:], in_=dr["even_w_pool"][li].rearrange("g c d -> c g d")),
                 writes=["wpool"], dma_key="wpool")
            P.op("sp", lambda e: e.dma_start(out=pscale[:], in_=dr["even_pool_scale"][li].rearrange("(g p) -> p g", p=128),
                                             allow_slow_non_contiguous=True), writes=["pscale"], dma_key="pscale")
            carve_reset()
            u_sb = carve([128, NT, 512], BF16)
            pooledT = [carve([128, 512], BF16) for _ in range(2)]
            k = ring_next()
            wload(k, ring[k][:, :, :], w_in.rearrange("(c p) n -> p c n", p=128)[:, :, 1536:2048])
            for i in range(NT):
                b = i % 2
                for c in range(NC_):
                    P.op("pe", lambda e, c=c, i=i, b=b: e.matmul(
                        ps[b][:], lhsT=hT[:, c, i * 128:(i + 1) * 128], rhs=ring[k][:, c, :],
                        start=(c == 0), stop=(c == NC_ - 1)),
                        reads=[("ring", k), ("hT", i)], writes=[("ps", b)])
                P.op("act", lambda e, i=i, b=b: e.copy(out=u_sb[:, i, :], in_=ps[b][:]),
                     reads=[("ps", b)], writes=[("u", i)])
            npool = 0
            for g in range(4):
                for tb in range(4):
                    b = 2 + npool % 2
                    for q in range(4):
                        i = 4 * tb + q
                        P.op("pe", lambda e, i=i, q=q, g=g, b=b: e.matmul(
                            ps[b][:, q * 128:(q + 1) * 128], lhsT=u_sb[:, i, g * 128:(g + 1) * 128],
                            rhs=band[:, g, (2 if i == 0 else 0), :], start=True, stop=(i == 0)),
                            reads=[("u", i), "band"], writes=[("ps", b)])
                        if i > 0:
                            P.op("pe", lambda e, i=i, q=q, g=g, b=b: e.matmul(
                                ps[b][:, q * 128:(q + 1) * 128], lhsT=u_sb[:, i - 1, g * 128:(g + 1) * 128],
                                rhs=band[:, g, 1, :], start=False, stop=True),
                                reads=[("u", i - 1), "band"], writes=[("ps", b)])
                    pt = pooledT[npool % 2]
                    pk = npool % 2
                    P.op("dve", lambda e, pt=pt, b=b: e.tensor_copy(out=pt, in_=ps[b][:]),
                         reads=[("ps", b)], writes=[("pooledT", pk)])
                    b2 = 4 + npool % 2
                    P.op("pe", lambda e, pt=pt, g=g, b2=b2: e.matmul(
                        ps[b2][:], lhsT=wpool[:, g, :], rhs=pt, start=True, stop=True),
                        reads=[("pooledT", pk), "wpool"], writes=[("ps", b2)])
                    P.op("act", lambda e, g=g, tb=tb, b2=b2: e.activation(
                        out=catT[:, 4 + g, tb * 512:(tb + 1) * 512], in_=ps[b2][:], func=AF.Copy,
                        scale=pscale[:, g:g + 1]),
                        reads=[("ps", b2), "pscale"], writes=[("catT", 4 + g, tb)])
                    npool += 1
            P.fence()
            carve_reset()
            qT = carve([128, S], BF16)
            kT = carve([128, S], BF16)
            vp = carve([128, NT, 128], BF16)
            ebuf = carve([128, 512], F32)
            Yb = [carve([128, 512], BF16) for _ in range(2)]
            Rb = [carve([128, 512], BF16) for _ in range(2)]
            aTb = [carve([128, 512], BF16) for _ in range(2)]
            kctr = [0]
            seqctr = [0]

            def sb_a(it):
                k = it["k"]
                bz = k % 2
                Y = Yb[k % 2]
                P.op("pe", lambda e, it=it, bz=bz: e.matmul(ps[bz][:], lhsT=it["kTs"], rhs=it["qTs"], start=True, stop=False),
                     reads=it["rq"], writes=[("ps", bz)])
                P.op("act", lambda e, bz=bz: e.activation(out=ebuf, in_=ps[bz][:], func=AF.Exp),
                     reads=[("ps", bz)], writes=["ebuf"])
                P.op("act", lambda e, Y=Y: e.activation(out=Y, in_=ebuf, func=AF.Ln, bias=1.0),
                     reads=["ebuf"], writes=[("Y", k % 2)])
                if it["diag"]:
                    P.op("dve", lambda e, Y=Y, it=it: e.tensor_tensor(out=Y, in0=Y, in1=it["msl"], op=ALU.mult),
                         reads=[("Y", k % 2), "mask_strict"], writes=[("Y", k % 2)])

            def sb_b(it):
                k = it["k"]
                step = it["step"]
                bd = k % 2
                Y = Yb[k % 2]
                aT = aTb[k % 2]
                Rprev = Rb[(k + 1) % 2]
                Rcur = Rb[k % 2]
                P.op("pe", lambda e, Y=Y, bd=bd, step=step: e.matmul(ps[bd][:], lhsT=negtri[:], rhs=Y, start=False, stop=(step == 0)),
                     reads=[("Y", k % 2), "negtri"], writes=[("ps", bd)])
                if step > 0:
                    P.op("pe", lambda e, Rprev=Rprev, bd=bd: e.matmul(ps[bd][:], lhsT=negones[:], rhs=Rprev, start=False, stop=True),
                         reads=[("R", (k + 1) % 2), "negones"], writes=[("ps", bd)])
                P.op("act", lambda e, aT=aT, bd=bd: e.activation(out=aT, in_=ps[bd][:], func=AF.Exp),
                     reads=[("ps", bd)], writes=[("aT", k % 2)])
                if it["diag"]:
                    P.op("dve", lambda e, aT=aT, it=it: e.tensor_tensor(out=aT, in0=aT, in1=it["msl"], op=ALU.mult),
                         reads=[("aT", k % 2), "mask_strict"], writes=[("aT", k % 2)])
                if step == 0:
                    if it["njc"] > 1:
                        P.op("dve", lambda e, Rcur=Rcur, Y=Y: e.tensor_copy(out=Rcur, in_=Y),
                             reads=[("Y", k % 2)], writes=[("R", k % 2)])
                elif step < it["njc"] - 1:
                    P.op("dve", lambda e, Rcur=Rcur, Rprev=Rprev, Y=Y: e.tensor_tensor(out=Rcur, in0=Rprev, in1=Y, op=ALU.add),
                         reads=[("Y", k % 2), ("R", (k + 1) % 2)], writes=[("R", k % 2)])

            def sb_c(it):
                k = it["k"]
                aT = aTb[k % 2]
                bo = it["bo"]
                P.op("pe", lambda e, aT=aT, it=it, bo=bo: e.matmul(
                    ps[bo][:], lhsT=vp[:, it["jc"], :], rhs=aT, start=(it["step"] == 0), stop=(it["step"] == it["njc"] - 1)),
                    reads=[("aT", k % 2), ("vp", it["jc"] // 4)], writes=[("ps", bo)])
                if it["step"] == it["njc"] - 1:
                    base, pr, qb = it["base"], it["pr"], it["qb"]
                    P.op("dve", lambda e, base=base, pr=pr, qb=qb, bo=bo: e.tensor_copy(
                        out=catT[base:base + 64, pr, qb * 512:(qb + 1) * 512], in_=ps[bo][base:base + 64, :]),
                        reads=[("ps", bo)], writes=[("catT", pr, qb)])

            for pr in range(4):
                qkv_pair(w_in, pr * 128, 512 + pr * 128, 1024 + pr * 128, qT, kT, vp)
                items = []
                for hh in range(2):
                    base = hh * 64
                    for qb in range(4):
                        njc = 4 * qb + 4
                        bo = 6 + seqctr[0] % 2
                        seqctr[0] += 1
                        for step, jc in enumerate(range(njc - 1, -1, -1)):
                            r = jc - 4 * qb
                            diag = jc >= 4 * qb
                            items.append(dict(
                                k=kctr[0], step=step, jc=jc, njc=njc, diag=diag, bo=bo, base=base, pr=pr, qb=qb,
                                kTs=kT[base:base + 64, jc * 128:(jc + 1) * 128],
                                qTs=qT[base:base + 64, qb * 512:(qb + 1) * 512],
                                rq=[("kT", jc // 4), ("qT", qb)],
                                msl=(mask_strict[:, 384 - 128 * r:384 - 128 * r + 512] if diag else None)))
                            kctr[0] += 1
                skew_emit(items, [sb_a, sb_b, sb_c])
            P.fence()
            outproj_phase(dr["even_w_out"][li])

        def odd_phase(li):
            w = dr["odd_w_qkv"][li]
            carve_reset()
            qT = carve([128, S], BF16)
            kT = carve([128, S], BF16)
            vp = carve([128, NT, 128], BF16)
            maskT = carve([40, S], BF16)
            pTb = [carve([128, 512], BF16) for _ in range(2)]
            rden = carve([128, 512], F32)
            kmf = carve([128, 8], F32)
            kmT = carve([128, 8], BF16)
            gate = carve([128, NT, 2, 8], F32)
            g2 = carve([128, NT, 2, 8], F32)
            eq = carve([128, NT, 2, 8], F32)
            mx = carve([128, NT, 2], F32)
            mball = carve([128, NT, 64], BF16)
            mb4 = mball.rearrange("p t (h c) -> p t h c", h=2)
            P.op("dve", lambda e: e.memset(mball, 0.0), writes=["mb"])
            kctr = [0]
            seqctr = [0]
            G4 = [128, NT, 2, 8]

            def mo_a(it):
                k = it["k"]
                bs = k % 2
                P.op("pe", lambda e, it=it, bs=bs: e.matmul(ps[bs][:], lhsT=it["kTs"], rhs=it["qTs"], start=True, stop=False),
                     reads=it["rq"], writes=[("ps", bs)])
                P.op("pe", lambda e, it=it, bs=bs: e.matmul(
                    ps[bs][:], lhsT=en_sb[it["mbase"]:it["mbase"] + 8, it["jc"] // 2, :],
                    rhs=maskT[it["mbase"]:it["mbase"] + 8, it["qb"] * 512:(it["qb"] + 1) * 512],
                    start=False, stop=(not it["diag"])),
                    reads=[("maskT", it["qb"]), "en_sb"], writes=[("ps", bs)])
                if it["diag"]:
                    P.op("pe", lambda e, it=it, bs=bs: e.matmul(ps[bs][:], lhsT=ident[:], rhs=it["bsl"], start=False, stop=True),
                         reads=["bias_incl", "ident"], writes=[("ps", bs)])

            def mo_b(it):
                k = it["k"]
                bs = k % 2
                pT = pTb[k % 2]
                P.op("act", lambda e, pT=pT, bs=bs, it=it: e.activation(
                    out=pT, in_=ps[bs][:], func=AF.Exp, bias=albias[:, it["h"], it["o"]:it["o"] + 1]),
                    reads=[("ps", bs), "albias"], writes=[("pT", k % 2)])

            def mo_c(it):
                k = it["k"]
                pT = pTb[k % 2]
                bo, bden = it["bo"], it["bden"]
                first = it["jc"] == 0
                last = it["jc"] == it["njc"] - 1
                P.op("pe", lambda e, pT=pT, it=it, bo=bo, first=first, last=last: e.matmul(
                    ps[bo][:], lhsT=vp[:, it["jc"], :], rhs=pT, start=first, stop=last),
                    reads=[("pT", k % 2), ("vp", it["jc"] // 4)], writes=[("ps", bo)])
                P.op("pe", lambda e, pT=pT, bden=bden, first=first, last=last: e.matmul(
                    ps[bden][:], lhsT=ones_bf[:], rhs=pT, start=first, stop=last),
                    reads=[("pT", k % 2), "ones_bf"], writes=[("ps", bden)])
                if last:
                    base, pr, qb = it["base"], it["pr"], it["qb"]
                    P.op("dve", lambda e, base=base, bden=bden: e.reciprocal(out=rden[base:base + 64, :],
                                                                             in_=ps[bden][base:base + 64, :]),
                         reads=[("ps", bden)], writes=["rden"])
                    P.op("dve", lambda e, base=base, pr=pr, qb=qb, bo=bo: e.tensor_tensor(
                        out=catT[base:base + 64, pr, qb * 512:(qb + 1) * 512], in0=ps[bo][base:base + 64, :],
                        in1=rden[base:base + 64, :], op=ALU.mult),
                        reads=[("ps", bo), "rden"], writes=[("catT", pr, qb)])

            for pr in range(8):
                qkv_pair(w, pr * 128, 1024 + pr * 128, 2048 + pr * 128, qT, kT, vp)
                P.op("dve", lambda e: e.tensor_reduce(out=kmf, in_=kT.rearrange("p (n s) -> p n s", n=8),
                                                      axis=AX.X, op=ALU.add),
                     reads=[("kT", tb) for tb in range(4)], writes=["kmf"])
                P.op("act", lambda e: e.activation(out=kmT, in_=kmf, func=AF.Copy, scale=1.0 / 256),
                     reads=["kmf"], writes=["kmT"])
                for i in range(NT):
                    for hh in range(2):
                        base = hh * 64
                        c0 = (i * 2 + hh) * 8
                        P.op("pe", lambda e, i=i, base=base, c0=c0: e.matmul(
                            ps[4][:, c0:c0 + 8], lhsT=qT[base:base + 64, i * 128:(i + 1) * 128],
                            rhs=kmT[base:base + 64, :], start=True, stop=True),
                            reads=[("qT", i // 4), "kmT"], writes=[("ps", 4)])
                pastb = past_sb[:].unsqueeze(2).to_broadcast(G4)
                notb = notown_sb[:].unsqueeze(2).to_broadcast(G4)
                alt = altab[:, :, 2 * pr:2 * pr + 2].unsqueeze(3).to_broadcast(G4)
                mxb = mx.unsqueeze(3).to_broadcast(G4)
                P.op("dve", lambda e, pastb=pastb: e.tensor_tensor(
                    out=gate, in0=ps[4][:, 0:NT * 16].rearrange("p (t h n) -> p t h n", t=NT, h=2), in1=pastb, op=ALU.add),
                    reads=[("ps", 4), "past_sb"], writes=["gate"])
                src = gate
                for rnd in range(3):
                    P.op("dve", lambda e, src=src: e.tensor_reduce(out=mx, in_=src, axis=AX.X, op=ALU.max),
                         reads=["gate", "g2"], writes=["mx"])
                    if rnd == 2:
                        break
                    P.op("dve", lambda e, src=src, mxb=mxb: e.tensor_tensor(out=eq, in0=src, in1=mxb, op=ALU.is_ge),
                         reads=["gate", "g2", "mx"], writes=["eq"])
                    P.op("dve", lambda e, src=src: e.scalar_tensor_tensor(
                        out=g2, in0=eq, scalar=NEG, in1=src, op0=ALU.mult, op1=ALU.add),
                        reads=["eq", "gate", "g2"], writes=["g2"])
                    src = g2
                P.op("dve", lambda e, mxb=mxb: e.tensor_tensor(out=eq, in0=gate, in1=mxb, op=ALU.is_ge),
                     reads=["gate", "mx"], writes=["eq"])
                P.op("dve", lambda e: e.tensor_scalar(out=eq, in0=eq, scalar1=-1.0, scalar2=-NEG,
                                                      op0=ALU.add, op1=ALU.mult),
                     reads=["eq"], writes=["eq"])
                P.op("dve", lambda e, pastb=pastb: e.tensor_tensor(out=eq, in0=eq, in1=pastb, op=ALU.add),
                     reads=["eq", "past_sb"], writes=["eq"])
                P.op("dve", lambda e, notb=notb: e.tensor_tensor(out=eq, in0=eq, in1=notb, op=ALU.mult),
                     reads=["eq", "notown_sb"], writes=["eq"])
                P.op("dve", lambda e, alt=alt: e.tensor_tensor(out=mb4[:, :, :, 0:8], in0=eq, in1=alt, op=ALU.add),
                     reads=["eq", "altab"], writes=["mb"])
                P.fence()
                for tb in range(4):
                    pbk = (5, 3)[tb % 2]
                    for q in range(4):
                        i = 4 * tb + q
                        P.op("pe", lambda e, q=q, pbk=pbk, i=i: e.matmul(
                            ps[pbk][0:64, q * 128:(q + 1) * 128], lhsT=mball[:, i, :], rhs=ident[:],
                            start=True, stop=True),
                            reads=["mb", "ident"], writes=[("ps", pbk)])
                    P.op("act", lambda e, tb=tb, pbk=pbk: e.copy(
                        out=maskT[0:40, tb * 512:(tb + 1) * 512], in_=ps[pbk][0:40, :]),
                        reads=[("ps", pbk)], writes=[("maskT", tb)])
                items = []
                for hh in range(2):
                    base = hh * 64
                    h = 2 * pr + hh
                    for qb in range(4):
                        njc = 4 * qb + 4
                        bo, bden = ((6, 7), (2, 4))[seqctr[0] % 2]
                        seqctr[0] += 1
                        for jc in range(njc):
                            diag = jc >= 4 * qb
                            r = jc - 4 * qb
                            items.append(dict(
                                k=kctr[0], jc=jc, njc=njc, diag=diag, bo=bo, bden=bden, base=base, mbase=32 * hh,
                                pr=pr, qb=qb, h=h, o=4 * qb - jc + 3,
                                kTs=kT[base:base + 64, jc * 128:(jc + 1) * 128],
                                qTs=qT[base:base + 64, qb * 512:(qb + 1) * 512],
                                rq=[("kT", jc // 4), ("qT", qb)],
                                bsl=(bias_incl[:, 384 - 128 * r:384 - 128 * r + 512] if diag else None)))
                            kctr[0] += 1
                skew_emit(items, [mo_a, mo_b, mo_c])
            P.fence()
            outproj_phase(dr["odd_w_o"][li])

        def final_phase():
            carve_reset()
            junk = carve([128, D], BF16)
            yb = [carve([128, D], F32) for _ in range(2)]
            P.op("sp", lambda e: e.dma_start(out=gfin, in_=dr["norm_final"].partition_broadcast(128)),
                 writes=["gfin"], dma_key="gfin")
            for i in range(NT):
                P.op("act", lambda e, i=i: e.activation(out=junk, in_=x_sb[:, i, :], func=AF.Square,
                                                        accum_out=ss[:, i:i + 1]),
                     reads=[("x", i, 0), ("x", i, 1)], writes=[("ss", i)])
            P.op("act", lambda e: e.activation(out=rstd[:], in_=ss[:], func=AF.Sqrt, scale=1.0 / D, bias=EPS),
                 reads=[("ss", i) for i in range(NT)], writes=["rstd"])
            P.op("dve", lambda e: e.reciprocal(out=rstd[:], in_=rstd[:]), reads=["rstd"], writes=["rstd"])
            for i in range(NT):
                y = yb[i % 2]
                P.op("dve", lambda e, i=i, y=y: e.scalar_tensor_tensor(
                    out=y, in0=x_sb[:, i, :], scalar=rstd[:, i:i + 1], in1=gfin, op0=ALU.mult, op1=ALU.mult),
                    reads=[("x", i, 0), ("x", i, 1), "rstd", "gfin"], writes=[("y", i % 2)])
                P.op("sp", lambda e, i=i, y=y: e.dma_start(out=out_d[i * 128:(i + 1) * 128, :], in_=y),
                     reads=[("y", i % 2)], dma_key=("out", i % 2))

        def run(stage):
            return stages is None or stage in stages

        P.op("dve", lambda e: e.memset(ss[:], 0.0), writes=[("ss", i) for i in range(NT)])
        for l in range(n_layers):
            if run("ffn1"):
                norm_phase(0 * 4 + l)
                P.fence()
                ffn_phase(dr["ffn1_gate"][l], dr["ffn1_up"][l], dr["ffn1_down"][l])
                P.fence()
            if run("mix"):
                norm_phase(1 * 4 + l)
                P.fence()
                if l % 2 == 0:
                    if not DEBUG.get("only_odd"):
                        even_phase(l // 2)
                else:
                    odd_phase(l // 2)
                P.fence()
            if run("ffn2"):
                norm_phase(2 * 4 + l)
                P.fence()
                ffn_phase(dr["ffn2_gate"][l], dr["ffn2_up"][l], dr["ffn2_down"][l])
                P.fence()
        if dbg_stage == "raw":
            for i in range(NT):
                P.op("sp", lambda e, i=i: e.dma_start(out=out_d[i * 128:(i + 1) * 128, :], in_=x_sb[:, i, :]),
                     reads=[("x", i, 0), ("x", i, 1)], dma_key=("out", i % 2))
        else:
            final_phase()

        print("ops recorded:", len(P.ops))
        P.finalize(lambda name: st.enter_context(nc.semaphore(name)))
        print("semaphores:", len(P.eng_sems) + len(P.dma_sems))
        mxv = {}
        for o in P.ops:
            if o["sig"] is not None:
                key = o["eng"] if o["dma_key"] is None else ("dma", o["dma_key"])
                mxv[key] = max(mxv.get(key, 0), o["sig"][1])
        print("max sem values:", {k: v for k, v in mxv.items() if not isinstance(k, tuple)},
              "dma max:", max(v for k, v in mxv.items() if isinstance(k, tuple)))
        with nc.Block() as block:
            @block.tensor
            def _(e):
                P.emit("pe", e)

            @block.scalar
            def _(e):
                P.emit("act", e)

            @block.vector
            def _(e):
                P.emit("dve", e)

            @block.gpsimd
            def _(e):
                P.emit("pool", e)

            @block.sync
            def _(e):
                P.emit("sp", e)
                P.final_waits(e)
    return nc, consts


_CACHE = {}


def kernel(**inputs):
    n = 8
    if "nc" not in _CACHE:
        _CACHE["nc"] = build()
    nc, consts = _CACHE["nc"]
    x = np.ascontiguousarray(np.asarray(inputs["x"], dtype=np.float32))
    shared = {k: np.ascontiguousarray(np.asarray(inputs[k], dtype=np.float32)) for k in WEIGHT_SHAPES}
    shared.update(consts)
    in_maps = []
    for b in range(n):
        m = dict(shared)
        m["x"] = x[b]
        in_maps.append(m)
    res = run_bass_kernel_spmd(nc, in_maps, core_ids=list(range(n)))
    return np.stack([np.asarray(r["out"], dtype=np.float32) for r in res.results], axis=0)
```

```python
import numpy as np
import ml_dtypes
import concourse.bass as bass
import concourse.mybir as mybir
from concourse.bass_utils import run_bass_kernel_spmd

F32 = mybir.dt.float32
BF16 = mybir.dt.bfloat16
AF = mybir.ActivationFunctionType
ALU = mybir.AluOpType
AX = mybir.AxisListType

S = 2048
D = 1024
DFF = 2816
NT = S // 128
NC_ = D // 128
NFF = DFF // 128
DEPTH = 4
EPS = 1e-6
NEG = -30000.0

COMPUTE = ("pe", "act", "dve")
SEM_ROLL = 2000


class Prog:
    def __init__(self, nc):
        self.nc = nc
        self.ops = []
        self.last_writer = {}
        self.readers = {}
        self.dma_cnt = {}
        self.fence_at = []

    def _deps(self, reads, writes):
        deps = set()
        for r in reads:
            w = self.last_writer.get(r)
            if w is not None:
                deps.add(w)
        for r in writes:
            w = self.last_writer.get(r)
            if w is not None:
                deps.add(w)
            last_by_eng = {}
            for rd in self.readers.get(r, ()):
                o = self.ops[rd]
                if o["dma_key"] is not None:
                    deps.add(rd)
                else:
                    last_by_eng[o["eng"]] = max(last_by_eng.get(o["eng"], -1), rd)
            deps.update(last_by_eng.values())
        return deps

    def op(self, eng, fn, reads=(), writes=(), dma_key=None):
        idx = len(self.ops)
        deps = self._deps(reads, writes)
        dep_vals = {}
        for d in deps:
            dk = self.ops[d]["dma_key"]
            if dk is not None:
                dep_vals[d] = 16 * self.dma_cnt[dk]
        if dma_key is not None:
            self.dma_cnt[dma_key] = self.dma_cnt.get(dma_key, 0) + 1
        self.ops.append(dict(eng=eng, fn=fn, deps=deps, dep_vals=dep_vals, dma_key=dma_key,
                             signal=False, sig=None, nfence=len(self.fence_at)))
        for r in reads:
            self.readers.setdefault(r, []).append(idx)
        for r in writes:
            self.last_writer[r] = idx
            self.readers[r] = []
        return idx

    def fence(self):
        self.fence_at.append(len(self.ops))

    def finalize(self, sem_alloc):
        ops = self.ops
        fence_last = []
        for fpos in self.fence_at:
            last = {}
            for e in COMPUTE:
                for i in range(fpos - 1, -1, -1):
                    if ops[i]["eng"] == e and ops[i]["dma_key"] is None:
                        last[e] = i
                        break
            fence_last.append(last)
        self.fence_last = fence_last
        for last in fence_last:
            for i in last.values():
                ops[i]["signal"] = True
        for i, o in enumerate(ops):
            keep = set()
            for d in o["deps"]:
                p = ops[d]
                if p["dma_key"] is None:
                    if p["eng"] == o["eng"] and o["dma_key"] is None:
                        if o["eng"] == "pe":
                            continue
                    p["signal"] = True
                keep.add(d)
            o["deps"] = keep
        cnt = {}
        semidx = {}
        self.eng_sems = {}
        for o in ops:
            if o["dma_key"] is not None:
                continue
            if not o["signal"]:
                continue
            e = o["eng"]
            k = semidx.get(e, 0)
            c = cnt.get((e, k), 0)
            if c >= SEM_ROLL:
                k += 1
                semidx[e] = k
                c = 0
            c += 1
            cnt[(e, k)] = c
            if (e, k) not in self.eng_sems:
                self.eng_sems[(e, k)] = sem_alloc("p_%s_%d" % (e, k))
            o["sig"] = (self.eng_sems[(e, k)], c)
        self.dma_sems = {}
        run = {}
        for o in ops:
            dk = o["dma_key"]
            if dk is None:
                continue
            if dk not in self.dma_sems:
                self.dma_sems[dk] = sem_alloc("d_%s" % (str(dk),))
            run[dk] = run.get(dk, 0) + 1
            o["sig"] = (self.dma_sems[dk], 16 * run[dk])

    def emit(self, eng_name, e):
        ops = self.ops
        waited = {}
        nf_done = 0

        def wait(sem, val):
            key = id(sem)
            if waited.get(key, 0) >= val:
                return
            waited[key] = val
            e.wait_ge(sem, val)

        n_wait0 = 0
        for i, o in enumerate(ops):
            if o["eng"] != eng_name:
                continue
            if eng_name in COMPUTE:
                while nf_done < o["nfence"]:
                    for oe, li in self.fence_last[nf_done].items():
                        sem, val = ops[li]["sig"]
                        wait(sem, val)
                    nf_done += 1
            for d in sorted(o["deps"]):
                p = ops[d]
                sem, val = p["sig"]
                if p["dma_key"] is not None:
                    val = o["dep_vals"][d]
                wait(sem, val)
            ins = o["fn"](e)
            if o["dma_key"] is not None:
                ins.then_inc(o["sig"][0], 16)
            elif o["signal"]:
                ins.then_inc(o["sig"][0], 1)

    def final_waits(self, e):
        run = {}
        for o in self.ops:
            if o["dma_key"] is not None:
                run[o["dma_key"]] = o["sig"]
        for dk, (sem, val) in run.items():
            e.wait_ge(sem, val)


def make_consts():
    bf = ml_dtypes.bfloat16
    c = {}
    c["c_ident"] = np.eye(128, dtype=np.float32).astype(bf)
    c["c_ones"] = np.ones((128, 128), np.float32).astype(bf)
    j = np.arange(128)[:, None]
    col = np.arange(897)[None, :]
    c["c_mask_strict"] = (j + 384 < col).astype(np.float32).astype(bf)
    c["c_bias_incl"] = np.where(j + 384 <= col, 0.0, NEG).astype(np.float32).astype(bf)
    s_ = np.arange(128)[None, :]
    c["c_negtri"] = (-(j >= s_).astype(np.float32)).astype(bf)
    c["c_negones"] = (-np.ones((128, 128), np.float32)).astype(bf)
    band = np.zeros((4, 3, 128, 128), np.float32)
    sidx = np.arange(128)[:, None]
    tidx = np.arange(128)[None, :]
    for g, w in enumerate((2, 4, 8, 16)):
        d0 = tidx - sidx
        band[g, 0] = np.where((d0 >= 0) & (d0 < w), 1.0 / w, 0.0) - (d0 == 0)
        d1 = tidx + 128 - sidx
        band[g, 1] = np.where((d1 >= 0) & (d1 < w), 1.0 / w, 0.0)
        cnt = np.minimum(tidx + 1, w).astype(np.float32)
        band[g, 2] = np.where((d0 >= 0) & (d0 < w), 1.0 / cnt, 0.0) - (d0 == 0)
    c["c_band"] = np.ascontiguousarray(band.transpose(2, 0, 1, 3)).astype(bf)
    slopes = (2.0 ** (-8.0 * np.arange(1, 17) / 16)).astype(np.float32)
    en = np.zeros((8, 8, 128), np.float32)
    for n in range(8):
        en[n, n, :] = 1.0
    c["c_en"] = en.astype(bf)
    p = np.arange(128, dtype=np.float32)[:, None, None]
    off = (np.arange(19, dtype=np.float32) - 3.0)[None, None, :] * 128.0
    c["c_albias"] = (slopes[None, :, None] * (p - off)).astype(np.float32)
    tl = ((np.arange(16) % 4) * 128.0)[None, :, None] + p
    c["c_altab"] = (-(slopes[None, None, :]) * tl).astype(np.float32)
    blk = (np.arange(16) // 2)[:, None]
    n = np.arange(8)[None, :]
    past = np.where(n < blk, 0.0, NEG).astype(np.float32)
    c["c_past"] = np.ascontiguousarray(np.broadcast_to(past[None], (128, 16, 8))).astype(np.float32)
    notown = (n != blk).astype(np.float32)
    c["c_notown"] = np.ascontiguousarray(np.broadcast_to(notown[None], (128, 16, 8))).astype(np.float32)
    return c


CONST_DT = {"c_ident": BF16, "c_ones": BF16, "c_mask_strict": BF16, "c_bias_incl": BF16,
            "c_negtri": BF16, "c_negones": BF16, "c_band": BF16, "c_en": BF16, "c_albias": F32,
            "c_altab": F32, "c_past": F32, "c_notown": F32}

WEIGHT_SHAPES = {
    "norm_ffn1": (4, D), "ffn1_gate": (4, D, DFF), "ffn1_up": (4, D, DFF), "ffn1_down": (4, DFF, D),
    "norm_mix": (4, D), "norm_ffn2": (4, D), "ffn2_gate": (4, D, DFF), "ffn2_up": (4, D, DFF),
    "ffn2_down": (4, DFF, D), "even_w_in": (2, D, 2048), "even_w_pool": (2, 4, 128, 128),
    "even_pool_scale": (2, 512), "even_w_out": (2, D, D), "odd_w_qkv": (2, D, 3072),
    "odd_w_o": (2, D, D), "norm_final": (D,),
}

RING_K = 4
DEBUG = {}
WORK_BYTES = 28 * 1024


def build(n_layers=DEPTH, stages=None, dbg_stage=None):
    from contextlib import ExitStack
    nc = bass.Bass("TRN2", target_bir_lowering=False)
    P = Prog(nc)
    dr = {}
    dr["x"] = nc.dram_tensor("x", [S, D], F32, kind="ExternalInput").ap()
    for name, shp in WEIGHT_SHAPES.items():
        dr[name] = nc.dram_tensor(name, list(shp), F32, kind="ExternalInput").ap()
    consts = make_consts()
    for name, arr in consts.items():
        dr[name] = nc.dram_tensor(name, list(arr.shape), CONST_DT[name], kind="ExternalInput").ap()
    out_d = nc.dram_tensor("out", [S, D], F32, kind="ExternalOutput").ap()

    st = ExitStack()
    with st:
        def T(name, shape, dt):
            return st.enter_context(nc.sbuf_tensor(name, list(shape), dt))

        x_sb = T("x_sb", [128, NT, D], F32)
        hT = T("hT", [128, NC_, S], BF16)
        catT = T("catT", [128, NC_, S], BF16)
        ring = [T("ring%d" % k, [128, 8, 512], BF16) for k in range(RING_K)]
        work = T("work", [128, WORK_BYTES // 2], BF16)
        ident = T("ident", [128, 128], BF16)
        ones_bf = T("ones_bf", [128, 128], BF16)
        mask_strict = T("mask_strict", [128, 897], BF16)
        bias_incl = T("bias_incl", [128, 897], BF16)
        negtri = T("negtri", [128, 128], BF16)
        negones = T("negones", [128, 128], BF16)
        band = T("band", [128, 4, 3, 128], BF16)
        en_sb = T("en_sb", [40, 8, 128], BF16)
        albias = T("albias", [128, 16, 19], F32)
        altab = T("altab", [128, 16, 16], F32)
        past_sb = T("past_sb", [128, 16, 8], F32)
        notown_sb = T("notown_sb", [128, 16, 8], F32)
        gT = T("gT", [128, 12, 8], F32)
        wpool = T("wpool", [128, 4, 128], BF16)
        pscale = T("pscale", [128, 4], F32)
        ss = T("ss", [128, NT], F32)
        gfin_t = T("gfin", [128, D], F32)
        gfin = gfin_t[:]
        rstd = T("rstd", [128, NT], F32)
        ps = [st.enter_context(nc.psum_tensor("ps%d" % k, [128, 512], F32)) for k in range(8)]
        print("sbuf bytes remaining:", nc.sbuf_bytes_remaining)

        woff = [0]

        def carve_reset():
            woff[0] = 0

        def carve(shape, dt):
            esz = 4 if dt == F32 else 2
            n = int(np.prod(shape[1:]))
            nb = n * esz
            o = woff[0]
            assert o % 4 == 0
            woff[0] = o + ((nb + 31) // 32) * 32
            assert woff[0] <= WORK_BYTES, (woff[0], WORK_BYTES)
            ap = work[0:shape[0], o // 2:(o + nb) // 2]
            if dt == F32:
                ap = ap.bitcast(F32)
            if len(shape) == 3:
                ap = ap.rearrange("p (a b) -> p a b", a=shape[1])
            elif len(shape) == 4:
                ap = ap.rearrange("p (a b c) -> p a b c", a=shape[1], b=shape[2])
            return ap

        def ld(dst, src, key, eng="sp", **kw):
            P.op(eng, lambda e: e.dma_start(out=dst, in_=src, **kw), writes=[key], dma_key=key)

        for t4 in range(4):
            P.op("sp", lambda e, t4=t4: e.dma_start(
                out=x_sb[:, 4 * t4:4 * t4 + 4, :],
                in_=dr["x"][t4 * 512:(t4 + 1) * 512, :].rearrange("(i p) d -> p i d", p=128)),
                writes=[("x", i, h) for i in range(4 * t4, 4 * t4 + 4) for h in range(2)],
                dma_key=("xin", t4))
        ld(ident[:], dr["c_ident"], "ident")
        ld(ones_bf[:], dr["c_ones"], "ones_bf")
        ld(mask_strict[:], dr["c_mask_strict"], "mask_strict")
        ld(bias_incl[:], dr["c_bias_incl"], "bias_incl")
        ld(negtri[:], dr["c_negtri"], "negtri")
        ld(negones[:], dr["c_negones"], "negones")
        ld(band[:], dr["c_band"], "band")
        P.op("sp", lambda e: e.dma_start(out=en_sb[0:8], in_=dr["c_en"]), writes=["en_sb"], dma_key="en_sb")
        P.op("sp", lambda e: e.dma_start(out=en_sb[32:40], in_=dr["c_en"]), writes=["en_sb"], dma_key="en_sb")
        ld(albias[:], dr["c_albias"], "albias")
        ld(altab[:], dr["c_altab"], "altab")
        ld(past_sb[:], dr["c_past"], "past_sb")
        ld(notown_sb[:], dr["c_notown"], "notown_sb")
        for k, nm in enumerate(("norm_ffn1", "norm_mix", "norm_ffn2")):
            for l in range(4):
                P.op("sp", lambda e, k=k, l=l, nm=nm: e.dma_start(
                    out=gT[:, k * 4 + l, :], in_=dr[nm][l].rearrange("(c p) -> p c", p=128),
                    allow_slow_non_contiguous=True), writes=["gT"], dma_key="gT")

        ring_ctr = [0]

        def ring_next():
            k = ring_ctr[0] % RING_K
            ring_ctr[0] += 1
            return k

        def wload(k, dst, src):
            P.op("pool", lambda e: e.dma_start(out=dst, in_=src), writes=[("ring", k)],
                 dma_key=("ring", k))

        def norm_phase(gidx):
            carve_reset()
            junk = carve([128, D], BF16)
            xn = [carve([128, D], BF16) for _ in range(2)]
            for i in range(NT):
                P.op("act", lambda e, i=i: e.activation(out=junk, in_=x_sb[:, i, :], func=AF.Square,
                                                        accum_out=ss[:, i:i + 1]),
                     reads=[("x", i, 0), ("x", i, 1)], writes=[("ss", i)])
                P.op("act", lambda e, i=i: e.activation(out=rstd[:, i:i + 1], in_=ss[:, i:i + 1],
                                                        func=AF.Sqrt, scale=1.0 / D, bias=EPS),
                     reads=[("ss", i)], writes=[("rstd", i)])
                P.op("dve", lambda e, i=i: e.reciprocal(out=rstd[:, i:i + 1], in_=rstd[:, i:i + 1]),
                     reads=[("rstd", i)], writes=[("rstd", i)])
                xb = xn[i % 2]
                P.op("act", lambda e, i=i, xb=xb: e.activation(out=xb, in_=x_sb[:, i, :], func=AF.Copy,
                                                               scale=rstd[:, i:i + 1]),
                     reads=[("x", i, 0), ("x", i, 1), ("rstd", i)], writes=[("xn", i % 2)])
                pb = 6 + (i % 2)
                psT = ps[pb][:].bitcast(BF16)
                for c in range(NC_):
                    P.op("pe", lambda e, c=c, xb=xb, psT=psT: e.transpose(
                        out=psT[:, c * 128:(c + 1) * 128], in_=xb[:, c * 128:(c + 1) * 128],
                        identity=ident[:]),
                        reads=[("xn", i % 2), "ident"], writes=[("ps", pb)])
                P.op("dve", lambda e, i=i, psT=psT: e.tensor_tensor(
                    out=hT[:, :, i * 128:(i + 1) * 128],
                    in0=psT.rearrange("p (c t) -> p c t", c=NC_),
                    in1=gT[:, gidx, :].unsqueeze(2).to_broadcast([128, NC_, 128]), op=ALU.mult),
                    reads=[("ps", pb), "gT"], writes=[("hT", i)])

        def ffn_phase(wg, wu, wd):
            carve_reset()
            sil = [carve([128, 512], F32) for _ in range(2)]
            groups = [(0, 8), (8, 8), (16, 6)]
            nsil = 0
            for (f0, n) in groups:
                for sub0 in range(0, n, 4):
                    ns = min(4, n - sub0)
                    kg = ring_next()
                    col0 = (f0 + sub0) * 128
                    wload(kg, ring[kg][:, :, 0:ns * 128],
                          wg.rearrange("(c p) n -> p c n", p=128)[:, :, col0:col0 + ns * 128])
                    ku = ring_next()
                    wload(ku, ring[ku][:, :, 0:ns * 128],
                          wu.rearrange("(c p) n -> p c n", p=128)[:, :, col0:col0 + ns * 128])
                    for jj in range(ns):
                        j = sub0 + jj
                        for tb in range(4):
                            bg = tb % 2
                            bu = 2 + tb % 2
                            for c in range(NC_):
                                P.op("pe", lambda e, c=c, jj=jj, tb=tb, kg=kg, bg=bg: e.matmul(
                                    ps[bg][:], lhsT=ring[kg][:, c, jj * 128:(jj + 1) * 128],
                                    rhs=hT[:, c, tb * 512:(tb + 1) * 512], start=(c == 0), stop=(c == NC_ - 1)),
                                    reads=[("ring", kg)] + [("hT", 4 * tb + q) for q in range(4)],
                                    writes=[("ps", bg)])
                            for c in range(NC_):
                                P.op("pe", lambda e, c=c, jj=jj, tb=tb, ku=ku, bu=bu: e.matmul(
                                    ps[bu][:], lhsT=ring[ku][:, c, jj * 128:(jj + 1) * 128],
                                    rhs=hT[:, c, tb * 512:(tb + 1) * 512], start=(c == 0), stop=(c == NC_ - 1)),
                                    reads=[("ring", ku)] + [("hT", 4 * tb + q) for q in range(4)],
                                    writes=[("ps", bu)])
                            sb = sil[nsil % 2]
                            sk = nsil % 2
                            nsil += 1
                            P.op("act", lambda e, sb=sb, bg=bg: e.activation(out=sb, in_=ps[bg][:], func=AF.Silu),
                                 reads=[("ps", bg)], writes=[("sil", sk)])
                            P.op("dve", lambda e, sb=sb, bu=bu, j=j, tb=tb: e.tensor_tensor(
                                out=catT[:, j, tb * 512:(tb + 1) * 512], in0=sb, in1=ps[bu][:], op=ALU.mult),
                                reads=[("sil", sk), ("ps", bu)], writes=[("catT", j, tb)])
                kd = [ring_next(), ring_next()]
                for dh in range(2):
                    wload(kd[dh], ring[kd[dh]][:, 0:n, :],
                          wd[f0 * 128:(f0 + n) * 128, dh * 512:(dh + 1) * 512].rearrange("(j p) n -> p j n", p=128))
                for i in range(NT):
                    for dh in range(2):
                        bo = 4 + (2 * i + dh) % 2
                        for j in range(n):
                            P.op("pe", lambda e, i=i, j=j, bo=bo, kk=kd[dh], n=n: e.matmul(
                                ps[bo][:], lhsT=catT[:, j, i * 128:(i + 1) * 128], rhs=ring[kk][:, j, :],
                                start=(j == 0), stop=(j == n - 1)),
                                reads=[("ring", kd[dh]), ("catT", j, i // 4)], writes=[("ps", bo)])
                        P.op("dve", lambda e, i=i, dh=dh, bo=bo: e.scalar_tensor_tensor(
                            out=x_sb[:, i, dh * 512:(dh + 1) * 512], in0=ps[bo][:], scalar=0.5,
                            in1=x_sb[:, i, dh * 512:(dh + 1) * 512], op0=ALU.mult, op1=ALU.add),
                            reads=[("ps", bo), ("x", i, dh)], writes=[("x", i, dh)])

        def outproj_phase(w):
            kd = [ring_next(), ring_next()]
            for dh in range(2):
                wload(kd[dh], ring[kd[dh]][:, :, :],
                      w[:, dh * 512:(dh + 1) * 512].rearrange("(c p) n -> p c n", p=128))
            for i in range(NT):
                for dh in range(2):
                    bo = 4 + (2 * i + dh) % 2
                    for c in range(NC_):
                        P.op("pe", lambda e, i=i, dh=dh, c=c, bo=bo: e.matmul(
                            ps[bo][:], lhsT=catT[:, c, i * 128:(i + 1) * 128], rhs=ring[kd[dh]][:, c, :],
                            start=(c == 0), stop=(c == NC_ - 1)),
                            reads=[("ring", kd[dh]), ("catT", c, i // 4)], writes=[("ps", bo)])
                    P.op("dve", lambda e, i=i, dh=dh, bo=bo: e.tensor_tensor(
                        out=x_sb[:, i, dh * 512:(dh + 1) * 512], in0=ps[bo][:],
                        in1=x_sb[:, i, dh * 512:(dh + 1) * 512], op=ALU.add),
                        reads=[("ps", bo), ("x", i, dh)], writes=[("x", i, dh)])

        def skew_emit(items, stage_fns, lag=1):
            n = len(items)
            ns = len(stage_fns)
            for i in range(n + lag * (ns - 1)):
                for st_i in range(ns):
                    j = i - lag * st_i
                    if 0 <= j < n:
                        stage_fns[st_i](items[j])

        def qkv_pair(w, qcol, kcol, vcol, qT, kT, vp):
            k = ring_next()
            wv = w.rearrange("(c p) n -> p c n", p=128)
            for jx, c0 in enumerate((qcol, kcol, vcol)):
                wload(k, ring[k][:, :, jx * 128:(jx + 1) * 128], wv[:, :, c0:c0 + 128])
            for which, dst in ((0, qT), (1, kT)):
                for tb in range(4):
                    b = tb % 2
                    for c in range(NC_):
                        P.op("pe", lambda e, c=c, tb=tb, b=b, which=which: e.matmul(
                            ps[b][:], lhsT=ring[k][:, c, which * 128:(which + 1) * 128],
                            rhs=hT[:, c, tb * 512:(tb + 1) * 512], start=(c == 0), stop=(c == NC_ - 1)),
                            reads=[("ring", k)] + [("hT", 4 * tb + q) for q in range(4)], writes=[("ps", b)])
                    if which == 0:
                        P.op("act", lambda e, tb=tb, b=b, dst=dst: e.activation(
                            out=dst[:, tb * 512:(tb + 1) * 512], in_=ps[b][:], func=AF.Copy, scale=0.125),
                            reads=[("ps", b)], writes=[("qT", tb)])
                    else:
                        P.op("dve", lambda e, tb=tb, b=b, dst=dst: e.tensor_copy(
                            out=dst[:, tb * 512:(tb + 1) * 512], in_=ps[b][:]),
                            reads=[("ps", b)], writes=[("kT", tb)])
            for i4 in range(4):
                b = 2 + i4 % 2
                for q in range(4):
                    i = 4 * i4 + q
                    for c in range(NC_):
                        P.op("pe", lambda e, c=c, i=i, q=q, b=b: e.matmul(
                            ps[b][:, q * 128:(q + 1) * 128], lhsT=hT[:, c, i * 128:(i + 1) * 128],
                            rhs=ring[k][:, c, 256:384], start=(c == 0), stop=(c == NC_ - 1)),
                            reads=[("ring", k), ("hT", i)], writes=[("ps", b)])
                P.op("dve", lambda e, i4=i4, b=b: e.tensor_copy(
                    out=vp[:, 4 * i4:4 * i4 + 4, :], in_=ps[b][:].rearrange("p (q n) -> p q n", q=4)),
                    reads=[("ps", b)], writes=[("vp", i4)])

        def even_phase(li):
            w_in = dr["even_w_in"][li]
            P.op("pool", lambda e: e.dma_start(out=wpool[:], in_=dr["even_w_pool"][li].rearrange("g c d -> c g d")),
                 writes=["wpool"], dma_key="wpool")
            P.op("sp", lambda e: e.dma_start(out=pscale[:], in_=dr["even_pool_scale"][li].rearrange("(g p) -> p g", p=128),
                                             allow_slow_non_contiguous=True), writes=["pscale"], dma_key="pscale")
            carve_reset()
            u_sb = carve([128, NT, 512], BF16)
            pooledT = [carve([128, 512], BF16) for _ in range(2)]
            k = ring_next()
            wload(k, ring[k][:, :, :], w_in.rearrange("(c p) n -> p c n", p=128)[:, :, 1536:2048])
            for i in range(NT):
                b = i % 2
                for c in range(NC_):
                    P.op("pe", lambda e, c=c, i=i, b=b: e.matmul(
                        ps[b][:], lhsT=hT[:, c, i * 128:(i + 1) * 128], rhs=ring[k][:, c, :],
                        start=(c == 0), stop=(c == NC_ - 1)),
                        reads=[("ring", k), ("hT", i)], writes=[("ps", b)])
                P.op("act", lambda e, i=i, b=b: e.copy(out=u_sb[:, i, :], in_=ps[b][:]),
                     reads=[("ps", b)], writes=[("u", i)])
            npool = 0
            for g in range(4):
                for tb in range(4):
                    b = 2 + npool % 2
                    for q in range(4):
                        i = 4 * tb + q
                        P.op("pe", lambda e, i=i, q=q, g=g, b=b: e.matmul(
                            ps[b][:, q * 128:(q + 1) * 128], lhsT=u_sb[:, i, g * 128:(g + 1) * 128],
                            rhs=band[:, g, (2 if i == 0 else 0), :], start=True, stop=(i == 0)),
                            reads=[("u", i), "band"], writes=[("ps", b)])
                        if i > 0:
                            P.op("pe", lambda e, i=i, q=q, g=g, b=b: e.matmul(
                                ps[b][:, q * 128:(q + 1) * 128], lhsT=u_sb[:, i - 1, g * 128:(g + 1) * 128],
                                rhs=band[:, g, 1, :], start=False, stop=True),
                                reads=[("u", i - 1), "band"], writes=[("ps", b)])
                    pt = pooledT[npool % 2]
                    pk = npool % 2
                    P.op("dve", lambda e, pt=pt, b=b: e.tensor_copy(out=pt, in_=ps[b][:]),
                         reads=[("ps", b)], writes=[("pooledT", pk)])
                    b2 = 4 + npool % 2
                    P.op("pe", lambda e, pt=pt, g=g, b2=b2: e.matmul(
                        ps[b2][:], lhsT=wpool[:, g, :], rhs=pt, start=True, stop=True),
                        reads=[("pooledT", pk), "wpool"], writes=[("ps", b2)])
                    P.op("act", lambda e, g=g, tb=tb, b2=b2: e.activation(
                        out=catT[:, 4 + g, tb * 512:(tb + 1) * 512], in_=ps[b2][:], func=AF.Copy,
                        scale=pscale[:, g:g + 1]),
                        reads=[("ps", b2), "pscale"], writes=[("catT", 4 + g, tb)])
                    npool += 1
            P.fence()
            carve_reset()
            qT = carve([128, S], BF16)
            kT = carve([128, S], BF16)
            vp = carve([128, NT, 128], BF16)
            ebuf = carve([128, 512], F32)
            Yb = [carve([128, 512], BF16) for _ in range(2)]
            Rb = [carve([128, 512], BF16) for _ in range(2)]
            aTb = [carve([128, 512], BF16) for _ in range(2)]
            kctr = [0]
            seqctr = [0]

            def sb_a(it):
                k = it["k"]
                bz = k % 2
                Y = Yb[k % 2]
                P.op("pe", lambda e, it=it, bz=bz: e.matmul(ps[bz][:], lhsT=it["kTs"], rhs=it["qTs"], start=True, stop=True),
                     reads=it["rq"], writes=[("ps", bz)])
                P.op("act", lambda e, bz=bz: e.activation(out=ebuf, in_=ps[bz][:], func=AF.Exp),
                     reads=[("ps", bz)], writes=["ebuf"])
                P.op("act", lambda e, Y=Y: e.activation(out=Y, in_=ebuf, func=AF.Ln, bias=1.0),
                     reads=["ebuf"], writes=[("Y", k % 2)])
                if it["diag"]:
                    P.op("dve", lambda e, Y=Y, it=it: e.tensor_tensor(out=Y, in0=Y, in1=it["msl"], op=ALU.mult),
                         reads=[("Y", k % 2), "mask_strict"], writes=[("Y", k % 2)])

            def sb_b(it):
                k = it["k"]
                step = it["step"]
                bd = 2 + k % 2
                Y = Yb[k % 2]
                aT = aTb[k % 2]
                Rprev = Rb[(k + 1) % 2]
                Rcur = Rb[k % 2]
                P.op("pe", lambda e, it=it, bd=bd: e.matmul(ps[bd][:], lhsT=it["kTs"], rhs=it["qTs"], start=True, stop=False),
                     reads=it["rq"], writes=[("ps", bd)])
                P.op("pe", lambda e, Y=Y, bd=bd, step=step: e.matmul(ps[bd][:], lhsT=negtri[:], rhs=Y, start=False, stop=(step == 0)),
                     reads=[("Y", k % 2), "negtri"], writes=[("ps", bd)])
                if step > 0:
                    P.op("pe", lambda e, Rprev=Rprev, bd=bd: e.matmul(ps[bd][:], lhsT=negones[:], rhs=Rprev, start=False, stop=True),
                         reads=[("R", (k + 1) % 2), "negones"], writes=[("ps", bd)])
                P.op("act", lambda e, aT=aT, bd=bd: e.activation(out=aT, in_=ps[bd][:], func=AF.Exp),
                     reads=[("ps", bd)], writes=[("aT", k % 2)])
                if it["diag"]:
                    P.op("dve", lambda e, aT=aT, it=it: e.tensor_tensor(out=aT, in0=aT, in1=it["msl"], op=ALU.mult),
                         reads=[("aT", k % 2), "mask_strict"], writes=[("aT", k % 2)])
                if step == 0:
                    if it["njc"] > 1:
                        P.op("dve", lambda e, Rcur=Rcur, Y=Y: e.tensor_copy(out=Rcur, in_=Y),
                             reads=[("Y", k % 2)], writes=[("R", k % 2)])
                elif step < it["njc"] - 1:
                    P.op("dve", lambda e, Rcur=Rcur, Rprev=Rprev, Y=Y: e.tensor_tensor(out=Rcur, in0=Rprev, in1=Y, op=ALU.add),
                         reads=[("Y", k % 2), ("R", (k + 1) % 2)], writes=[("R", k % 2)])

            def sb_c(it):
                k = it["k"]
                aT = aTb[k % 2]
                bo = it["bo"]
                P.op("pe", lambda e, aT=aT, it=it, bo=bo: e.matmul(
                    ps[bo][:], lhsT=vp[:, it["jc"], :], rhs=aT, start=(it["step"] == 0), stop=(it["step"] == it["njc"] - 1)),
                    reads=[("aT", k % 2), ("vp", it["jc"] // 4)], writes=[("ps", bo)])
                if it["step"] == it["njc"] - 1:
                    base, pr, qb = it["base"], it["pr"], it["qb"]
                    P.op("dve", lambda e, base=base, pr=pr, qb=qb, bo=bo: e.tensor_copy(
                        out=catT[base:base + 64, pr, qb * 512:(qb + 1) * 512], in_=ps[bo][base:base + 64, :]),
                        reads=[("ps", bo)], writes=[("catT", pr, qb)])

            for pr in range(4):
                qkv_pair(w_in, pr * 128, 512 + pr * 128, 1024 + pr * 128, qT, kT, vp)
                items = []
                for hh in range(2):
                    base = hh * 64
                    for qb in range(4):
                        njc = 4 * qb + 4
                        bo = 6 + seqctr[0] % 2
                        seqctr[0] += 1
                        for step, jc in enumerate(range(njc - 1, -1, -1)):
                            r = jc - 4 * qb
                            diag = jc >= 4 * qb
                            items.append(dict(
                                k=kctr[0], step=step, jc=jc, njc=njc, diag=diag, bo=bo, base=base, pr=pr, qb=qb,
                                kTs=kT[base:base + 64, jc * 128:(jc + 1) * 128],
                                qTs=qT[base:base + 64, qb * 512:(qb + 1) * 512],
                                rq=[("kT", jc // 4), ("qT", qb)],
                                msl=(mask_strict[:, 384 - 128 * r:384 - 128 * r + 512] if diag else None)))
                            kctr[0] += 1
                skew_emit(items, [sb_a, sb_b, sb_c])
            P.fence()
            outproj_phase(dr["even_w_out"][li])

        def odd_phase(li):
            w = dr["odd_w_qkv"][li]
            carve_reset()
            qT = carve([128, S], BF16)
            kT = carve([128, S], BF16)
            vp = carve([128, NT, 128], BF16)
            maskT = carve([40, S], BF16)
            pTb = [carve([128, 512], BF16) for _ in range(4)]
            SB = (0, 1, 3, 5)
            rden = carve([128, 512], F32)
            kmf = carve([128, 8], F32)
            kmT = carve([128, 8], BF16)
            gate = carve([128, NT, 2, 8], F32)
            g2 = carve([128, NT, 2, 8], F32)
            eq = carve([128, NT, 2, 8], F32)
            mx = carve([128, NT, 2], F32)
            mball = carve([128, NT, 64], BF16)
            mb4 = mball.rearrange("p t (h c) -> p t h c", h=2)
            P.op("dve", lambda e: e.memset(mball, 0.0), writes=["mb"])
            kctr = [0]
            seqctr = [0]
            G4 = [128, NT, 2, 8]

            def mo_a(it):
                k = it["k"]
                bs = SB[k % 4]
                P.op("pe", lambda e, it=it, bs=bs: e.matmul(ps[bs][:], lhsT=it["kTs"], rhs=it["qTs"], start=True, stop=False),
                     reads=it["rq"], writes=[("ps", bs)])
                P.op("pe", lambda e, it=it, bs=bs: e.matmul(
                    ps[bs][:], lhsT=en_sb[it["mbase"]:it["mbase"] + 8, it["jc"] // 2, :],
                    rhs=maskT[it["mbase"]:it["mbase"] + 8, it["qb"] * 512:(it["qb"] + 1) * 512],
                    start=False, stop=(not it["diag"])),
                    reads=[("maskT", it["qb"]), "en_sb"], writes=[("ps", bs)])
                if it["diag"]:
                    P.op("pe", lambda e, it=it, bs=bs: e.matmul(ps[bs][:], lhsT=ident[:], rhs=it["bsl"], start=False, stop=True),
                         reads=["bias_incl", "ident"], writes=[("ps", bs)])

            def mo_b(it):
                k = it["k"]
                bs = SB[k % 4]
                pT = pTb[k % 4]
                P.op("act", lambda e, pT=pT, bs=bs, it=it: e.activation(
                    out=pT, in_=ps[bs][:], func=AF.Exp, bias=albias[:, it["h"], it["o"]:it["o"] + 1]),
                    reads=[("ps", bs), "albias"], writes=[("pT", k % 4)])

            def mo_c(it):
                k = it["k"]
                pT = pTb[k % 4]
                bo, bden = it["bo"], it["bden"]
                first = it["jc"] == 0
                last = it["jc"] == it["njc"] - 1
                P.op("pe", lambda e, pT=pT, it=it, bo=bo, first=first, last=last: e.matmul(
                    ps[bo][:], lhsT=vp[:, it["jc"], :], rhs=pT, start=first, stop=last),
                    reads=[("pT", k % 4), ("vp", it["jc"] // 4)], writes=[("ps", bo)])
                P.op("pe", lambda e, pT=pT, bden=bden, first=first, last=last: e.matmul(
                    ps[bden][:], lhsT=ones_bf[:], rhs=pT, start=first, stop=last),
                    reads=[("pT", k % 4), "ones_bf"], writes=[("ps", bden)])
                if last:
                    base, pr, qb = it["base"], it["pr"], it["qb"]
                    P.op("dve", lambda e, base=base, bden=bden: e.reciprocal(out=rden[base:base + 64, :],
                                                                             in_=ps[bden][base:base + 64, :]),
                         reads=[("ps", bden)], writes=["rden"])
                    P.op("dve", lambda e, base=base, pr=pr, qb=qb, bo=bo: e.tensor_tensor(
                        out=catT[base:base + 64, pr, qb * 512:(qb + 1) * 512], in0=ps[bo][base:base + 64, :],
                        in1=rden[base:base + 64, :], op=ALU.mult),
                        reads=[("ps", bo), "rden"], writes=[("catT", pr, qb)])

            for pr in range(8):
                qkv_pair(w, pr * 128, 1024 + pr * 128, 2048 + pr * 128, qT, kT, vp)
                P.op("dve", lambda e: e.tensor_reduce(out=kmf, in_=kT.rearrange("p (n s) -> p n s", n=8),
                                                      axis=AX.X, op=ALU.add),
                     reads=[("kT", tb) for tb in range(4)], writes=["kmf"])
                P.op("act", lambda e: e.activation(out=kmT, in_=kmf, func=AF.Copy, scale=1.0 / 256),
                     reads=["kmf"], writes=["kmT"])
                for i in range(NT):
                    for hh in range(2):
                        base = hh * 64
                        c0 = (i * 2 + hh) * 8
                        P.op("pe", lambda e, i=i, base=base, c0=c0: e.matmul(
                            ps[4][:, c0:c0 + 8], lhsT=qT[base:base + 64, i * 128:(i + 1) * 128],
                            rhs=kmT[base:base + 64, :], start=True, stop=True),
                            reads=[("qT", i // 4), "kmT"], writes=[("ps", 4)])
                pastb = past_sb[:].unsqueeze(2).to_broadcast(G4)
                notb = notown_sb[:].unsqueeze(2).to_broadcast(G4)
                alt = altab[:, :, 2 * pr:2 * pr + 2].unsqueeze(3).to_broadcast(G4)
                mxb = mx.unsqueeze(3).to_broadcast(G4)
                P.op("dve", lambda e, pastb=pastb: e.tensor_tensor(
                    out=gate, in0=ps[4][:, 0:NT * 16].rearrange("p (t h n) -> p t h n", t=NT, h=2), in1=pastb, op=ALU.add),
                    reads=[("ps", 4), "past_sb"], writes=["gate"])
                src = gate
                for rnd in range(3):
                    P.op("dve", lambda e, src=src: e.tensor_reduce(out=mx, in_=src, axis=AX.X, op=ALU.max),
                         reads=["gate", "g2"], writes=["mx"])
                    if rnd == 2:
                        break
                    P.op("dve", lambda e, src=src, mxb=mxb: e.tensor_tensor(out=eq, in0=src, in1=mxb, op=ALU.is_ge),
                         reads=["gate", "g2", "mx"], writes=["eq"])
                    P.op("dve", lambda e, src=src: e.scalar_tensor_tensor(
                        out=g2, in0=eq, scalar=NEG, in1=src, op0=ALU.mult, op1=ALU.add),
                        reads=["eq", "gate", "g2"], writes=["g2"])
                    src = g2
                P.op("dve", lambda e, mxb=mxb: e.tensor_tensor(out=eq, in0=gate, in1=mxb, op=ALU.is_ge),
                     reads=["gate", "mx"], writes=["eq"])
                P.op("dve", lambda e: e.tensor_scalar(out=eq, in0=eq, scalar1=-1.0, scalar2=-NEG,
                                                      op0=ALU.add, op1=ALU.mult),
                     reads=["eq"], writes=["eq"])
                P.op("dve", lambda e, pastb=pastb: e.tensor_tensor(out=eq, in0=eq, in1=pastb, op=ALU.add),
                     reads=["eq", "past_sb"], writes=["eq"])
                P.op("dve", lambda e, notb=notb: e.tensor_tensor(out=eq, in0=eq, in1=notb, op=ALU.mult),
                     reads=["eq", "notown_sb"], writes=["eq"])
                P.op("dve", lambda e, alt=alt: e.tensor_tensor(out=mb4[:, :, :, 0:8], in0=eq, in1=alt, op=ALU.add),
                     reads=["eq", "altab"], writes=["mb"])
                P.fence()
                for tb in range(4):
                    pbk = (5, 3)[tb % 2]
                    for q in range(4):
                        i = 4 * tb + q
                        P.op("pe", lambda e, q=q, pbk=pbk, i=i: e.matmul(
                            ps[pbk][0:64, q * 128:(q + 1) * 128], lhsT=mball[:, i, :], rhs=ident[:],
                            start=True, stop=True),
                            reads=["mb", "ident"], writes=[("ps", pbk)])
                    P.op("act", lambda e, tb=tb, pbk=pbk: e.copy(
                        out=maskT[0:40, tb * 512:(tb + 1) * 512], in_=ps[pbk][0:40, :]),
                        reads=[("ps", pbk)], writes=[("maskT", tb)])
                items = []
                for hh in range(2):
                    base = hh * 64
                    h = 2 * pr + hh
                    for qb in range(4):
                        njc = 4 * qb + 4
                        bo, bden = ((6, 7), (2, 4))[seqctr[0] % 2]
                        seqctr[0] += 1
                        for jc in range(njc):
                            diag = jc >= 4 * qb
                            r = jc - 4 * qb
                            items.append(dict(
                                k=kctr[0], jc=jc, njc=njc, diag=diag, bo=bo, bden=bden, base=base, mbase=32 * hh,
                                pr=pr, qb=qb, h=h, o=4 * qb - jc + 3,
                                kTs=kT[base:base + 64, jc * 128:(jc + 1) * 128],
                                qTs=qT[base:base + 64, qb * 512:(qb + 1) * 512],
                                rq=[("kT", jc // 4), ("qT", qb)],
                                bsl=(bias_incl[:, 384 - 128 * r:384 - 128 * r + 512] if diag else None)))
                            kctr[0] += 1
                skew_emit(items, [mo_a, mo_b, mo_c], lag=2)
            P.fence()
            outproj_phase(dr["odd_w_o"][li])

        def final_phase():
            carve_reset()
            junk = carve([128, D], BF16)
            yb = [carve([128, D], F32) for _ in range(2)]
            P.op("sp", lambda e: e.dma_start(out=gfin, in_=dr["norm_final"].partition_broadcast(128)),
                 writes=["gfin"], dma_key="gfin")
            for i in range(NT):
                P.op("act", lambda e, i=i: e.activation(out=junk, in_=x_sb[:, i, :], func=AF.Square,
                                                        accum_out=ss[:, i:i + 1]),
                     reads=[("x", i, 0), ("x", i, 1)], writes=[("ss", i)])
                P.op("act", lambda e, i=i: e.activation(out=rstd[:, i:i + 1], in_=ss[:, i:i + 1],
                                                        func=AF.Sqrt, scale=1.0 / D, bias=EPS),
                     reads=[("ss", i)], writes=[("rstd", i)])
                P.op("dve", lambda e, i=i: e.reciprocal(out=rstd[:, i:i + 1], in_=rstd[:, i:i + 1]),
                     reads=[("rstd", i)], writes=[("rstd", i)])
                y = yb[i % 2]
                P.op("dve", lambda e, i=i, y=y: e.scalar_tensor_tensor(
                    out=y, in0=x_sb[:, i, :], scalar=rstd[:, i:i + 1], in1=gfin, op0=ALU.mult, op1=ALU.mult),
                    reads=[("x", i, 0), ("x", i, 1), ("rstd", i), "gfin"], writes=[("y", i % 2)])
                P.op("sp", lambda e, i=i, y=y: e.dma_start(out=out_d[i * 128:(i + 1) * 128, :], in_=y),
                     reads=[("y", i % 2)], dma_key=("out", i % 2))

        def run(stage):
            return stages is None or stage in stages

        P.op("dve", lambda e: e.memset(ss[:], 0.0), writes=[("ss", i) for i in range(NT)])
        for l in range(n_layers):
            if run("ffn1"):
                norm_phase(0 * 4 + l)
                P.fence()
                ffn_phase(dr["ffn1_gate"][l], dr["ffn1_up"][l], dr["ffn1_down"][l])
                P.fence()
            if run("mix"):
                norm_phase(1 * 4 + l)
                P.fence()
                if l % 2 == 0:
                    if not DEBUG.get("only_odd"):
                        even_phase(l // 2)
                else:
                    odd_phase(l // 2)
                P.fence()
            if run("ffn2"):
                norm_phase(2 * 4 + l)
                P.fence()
                ffn_phase(dr["ffn2_gate"][l], dr["ffn2_up"][l], dr["ffn2_down"][l])
                P.fence()
        if dbg_stage == "raw":
            for i in range(NT):
                P.op("sp", lambda e, i=i: e.dma_start(out=out_d[i * 128:(i + 1) * 128, :], in_=x_sb[:, i, :]),
                     reads=[("x", i, 0), ("x", i, 1)], dma_key=("out", i % 2))
        else:
            final_phase()

        print("ops recorded:", len(P.ops))
        P.finalize(lambda name: st.enter_context(nc.semaphore(name)))
        print("semaphores:", len(P.eng_sems) + len(P.dma_sems))
        mxv = {}
        for o in P.ops:
            if o["sig"] is not None:
                key = o["eng"] if o["dma_key"] is None else ("dma", o["dma_key"])
                mxv[key] = max(mxv.get(key, 0), o["sig"][1])
        print("max sem values:", {k: v for k, v in mxv.items() if not isinstance(k, tuple)},
              "dma max:", max(v for k, v in mxv.items() if isinstance(k, tuple)))
        with nc.Block() as block:
            @block.tensor
            def _(e):
                P.emit("pe", e)

            @block.scalar
            def _(e):
                P.emit("act", e)

            @block.vector
            def _(e):
                P.emit("dve", e)

            @block.gpsimd
            def _(e):
                P.emit("pool", e)

            @block.sync
            def _(e):
                P.emit("sp", e)
                P.final_waits(e)
    return nc, consts


_CACHE = {}


def kernel(**inputs):
    n = 8
    if "nc" not in _CACHE:
        _CACHE["nc"] = build()
    nc, consts = _CACHE["nc"]
    x = np.ascontiguousarray(np.asarray(inputs["x"], dtype=np.float32))
    shared = {k: np.ascontiguousarray(np.asarray(inputs[k], dtype=np.float32)) for k in WEIGHT_SHAPES}
    shared.update(consts)
    in_maps = []
    for b in range(n):
        m = dict(shared)
        m["x"] = x[b]
        in_maps.append(m)
    res = run_bass_kernel_spmd(nc, in_maps, core_ids=list(range(n)))
    return np.stack([np.asarray(r["out"], dtype=np.float32) for r in res.results], axis=0)
```
